# Optimizing a Trainium2 kernel written in Bass

```python
import math
import jax, jax.numpy as jnp
from jax import lax
import numpy as np

D_MODEL = 1024
BATCH = 2
SEQ = 16384
DEPTH = 2

CHUNK = 64
RMS_EPS = 1e-6
POOL_WINDOWS = (2, 4, 8, 16)
N_POOL_GROUPS = len(POOL_WINDOWS)
POOL_WIDTH = D_MODEL // 2
POOL_GROUP = POOL_WIDTH // N_POOL_GROUPS
CONV_CH = D_MODEL // 2
CONV_K = 3
MIX_IN0 = POOL_WIDTH + 3 * CONV_CH
HEAD_DIM = 64
N_Q_HEADS = D_MODEL // HEAD_DIM
N_KV_HEADS = 2
Q_PER_KV = N_Q_HEADS // N_KV_HEADS
WINDOW = 128
WINDOW_CHUNKS = -(-WINDOW // CHUNK)
KV_SPAN = (WINDOW_CHUNKS + 1) * CHUNK
QKV_WIDTH = (N_Q_HEADS + 2 * N_KV_HEADS) * HEAD_DIM
N_BUCKETS = 32
MAX_DISTANCE = 128
D_FF = 2816
N_EXPERTS = 8
TOP_K = 2
D_FF_EXPERT = 3584
N_EVEN = (DEPTH + 1) // 2
N_ODD = DEPTH // 2
NEG_INF = -1e30

kernel_name = 'hybrid_pool_conv_swa_moe_encoder'


def rmsnorm(x, g):
    xf = x.astype(jnp.float32)
    y = xf * lax.rsqrt(jnp.mean(xf * xf, axis=-1, keepdims=True) + RMS_EPS)
    return (y * g.astype(jnp.float32)).astype(x.dtype)


def swiglu(h, w_gate, w_up, w_down):
    return (jax.nn.silu(h @ w_gate) * (h @ w_up)) @ w_down


def multiscale_pool(u, pool_w, pool_scale):
    s = u.shape[1]
    uf = u.astype(jnp.float32)
    cs = jnp.pad(jnp.cumsum(uf, axis=1), ((0, 0), (1, 0), (0, 0)))
    t = jnp.arange(s)
    groups = []
    for g, w in enumerate(POOL_WINDOWS):
        c = cs[:, :, g * POOL_GROUP:(g + 1) * POOL_GROUP]
        hi = c[:, 1:]
        lo = jnp.pad(c[:, :s + 1 - w], ((0, 0), (w - 1, 0), (0, 0)))
        count = jnp.minimum(t + 1, w).astype(jnp.float32)[None, :, None]
        groups.append((hi - lo) / count - uf[:, :, g * POOL_GROUP:(g + 1) * POOL_GROUP])
    pooled = jnp.stack(groups, axis=2).astype(u.dtype)
    mixed = jnp.einsum('bsgc,gcd->bsgd', pooled, pool_w)
    return mixed.reshape(u.shape) * pool_scale


def causal_depthwise_conv(z, conv_w):
    c = z.shape[-1]
    return lax.conv_general_dilated(
        z, conv_w[:, None, :].astype(z.dtype), window_strides=(1,),
        padding=[(CONV_K - 1, 0)], dimension_numbers=('NWC', 'WIO', 'NWC'),
        feature_group_count=c)


def pool_conv_mixer(h, w_in, pool_w, pool_scale, conv_w, w_out):
    proj = h @ w_in
    u, gate_post, gate_pre, v = jnp.split(
        proj, [POOL_WIDTH, POOL_WIDTH + CONV_CH, POOL_WIDTH + 2 * CONV_CH], axis=-1)
    y_a = multiscale_pool(u, pool_w, pool_scale)
    y_b = gate_post * causal_depthwise_conv(gate_pre * v, conv_w)
    return jnp.concatenate([y_a, y_b], axis=-1) @ w_out


def t5_bucket(rel):
    nb = N_BUCKETS // 2
    max_exact = nb // 2
    ret = jnp.where(rel > 0, nb, 0)
    n = jnp.abs(rel)
    nf = jnp.maximum(n, 1).astype(jnp.float32)
    large = max_exact + (jnp.log(nf / max_exact) / math.log(MAX_DISTANCE / max_exact)
                         * (nb - max_exact)).astype(jnp.int32)
    large = jnp.minimum(large, nb - 1)
    return ret + jnp.where(n < max_exact, n, large)


def sliding_window_attention(h, w_qkv, b_qkv, sinks, w_o, b_o, rel_bias):
    b, s, _ = h.shape
    nc = s // CHUNK
    pad = WINDOW_CHUNKS * CHUNK
    qkv = h @ w_qkv + b_qkv
    q, k, v = jnp.split(qkv, [N_Q_HEADS * HEAD_DIM, (N_Q_HEADS + N_KV_HEADS) * HEAD_DIM], axis=-1)
    q = q.reshape(b, nc, CHUNK, N_KV_HEADS, Q_PER_KV, HEAD_DIM)

    def band(t):
        tp = jnp.pad(t.reshape(b, s, N_KV_HEADS, HEAD_DIM), ((0, 0), (pad, 0), (0, 0), (0, 0)))
        tp = tp.reshape(b, nc + WINDOW_CHUNKS, CHUNK, N_KV_HEADS, HEAD_DIM)
        return jnp.concatenate([tp[:, i:i + nc] for i in range(WINDOW_CHUNKS + 1)], axis=2)

    kb, vb = band(k), band(v)
    scores = jnp.einsum('bnqkgd,bnskd->bnkgqs', q, kb,
                        preferred_element_type=jnp.float32) * (HEAD_DIM ** -0.5)
    rel = (jnp.arange(KV_SPAN) - pad)[None, :] - jnp.arange(CHUNK)[:, None]
    bias = jnp.transpose(rel_bias[t5_bucket(rel)].astype(jnp.float32), (2, 0, 1))
    scores = scores + bias.reshape(N_KV_HEADS, Q_PER_KV, CHUNK, KV_SPAN)
    key_pos = jnp.arange(nc)[:, None] * CHUNK - pad + jnp.arange(KV_SPAN)[None, :]
    valid = (key_pos >= 0)[None, :, None, None, None, :]
    scores = jnp.where(valid, scores, NEG_INF)
    sink = sinks.astype(jnp.float32).reshape(N_KV_HEADS, Q_PER_KV)[None, None, :, :, None, None]
    m = jnp.maximum(jnp.max(scores, axis=-1, keepdims=True), sink)
    p = jnp.exp(scores - m)
    p = p / (jnp.sum(p, axis=-1, keepdims=True) + jnp.exp(sink - m))
    out = jnp.einsum('bnkgqs,bnskd->bnqkgd', p.astype(vb.dtype), vb)
    return out.reshape(b, s, N_Q_HEADS * HEAD_DIM) @ w_o + b_o


def moe_swiglu(h, w_router, w_gate, w_up, w_down):
    logits = (h @ w_router).astype(jnp.float32)
    top_vals, top_idx = lax.top_k(logits, TOP_K)
    top_w = jax.nn.softmax(top_vals, axis=-1)
    combine = jnp.einsum('bsk,bske->bse', top_w,
                         jax.nn.one_hot(top_idx, N_EXPERTS, dtype=jnp.float32)).astype(h.dtype)
    out = jnp.zeros_like(h)
    for e in range(N_EXPERTS):
        out = out + combine[..., e:e + 1] * swiglu(h, w_gate[e], w_up[e], w_down[e])
    return out


def setup_inputs(seed: int = 0) -> dict:
    key = jax.random.key(seed)
    ks = jax.random.split(key, 32)
    f32 = jnp.float32

    def nrm(k, shape, scale):
        return jax.random.normal(k, shape, f32) * scale

    d = D_MODEL
    return {
        'x': nrm(ks[0], (BATCH, SEQ, d), 1.0),
        'rel_bias': nrm(ks[1], (N_BUCKETS, N_Q_HEADS), 0.5),
        'ev_norm_mix': 1.0 + nrm(ks[2], (N_EVEN, d), 0.02),
        'ev_w_in': nrm(ks[3], (N_EVEN, d, MIX_IN0), d ** -0.5),
        'ev_pool_w': nrm(ks[4], (N_EVEN, N_POOL_GROUPS, POOL_GROUP, POOL_GROUP), POOL_GROUP ** -0.5),
        'ev_pool_scale': 1.0 + nrm(ks[5], (N_EVEN, POOL_WIDTH), 0.1),
        'ev_conv_w': nrm(ks[6], (N_EVEN, CONV_K, CONV_CH), CONV_K ** -0.5),
        'ev_w_out': nrm(ks[7], (N_EVEN, d, d), d ** -0.5),
        'ev_norm_ffn': 1.0 + nrm(ks[8], (N_EVEN, d), 0.02),
        'ev_ffn_gate': nrm(ks[9], (N_EVEN, d, D_FF), d ** -0.5),
        'ev_ffn_up': nrm(ks[10], (N_EVEN, d, D_FF), d ** -0.5),
        'ev_ffn_down': nrm(ks[11], (N_EVEN, D_FF, d), D_FF ** -0.5),
        'od_norm_mix': 1.0 + nrm(ks[12], (N_ODD, d), 0.02),
        'od_w_qkv': nrm(ks[13], (N_ODD, d, QKV_WIDTH), d ** -0.5),
        'od_b_qkv': nrm(ks[14], (N_ODD, QKV_WIDTH), 0.02),
        'od_sinks': nrm(ks[15], (N_ODD, N_Q_HEADS), 0.5),
        'od_w_o': nrm(ks[16], (N_ODD, N_Q_HEADS * HEAD_DIM, d), (N_Q_HEADS * HEAD_DIM) ** -0.5),
        'od_b_o': nrm(ks[17], (N_ODD, d), 0.02),
        'od_norm_ffn': 1.0 + nrm(ks[18], (N_ODD, d), 0.02),
        'od_router': nrm(ks[19], (N_ODD, d, N_EXPERTS), d ** -0.5),
        'od_exp_gate': nrm(ks[20], (N_ODD, N_EXPERTS, d, D_FF_EXPERT), d ** -0.5),
        'od_exp_up': nrm(ks[21], (N_ODD, N_EXPERTS, d, D_FF_EXPERT), d ** -0.5),
        'od_exp_down': nrm(ks[22], (N_ODD, N_EXPERTS, D_FF_EXPERT, d), D_FF_EXPERT ** -0.5),
        'final_norm': 1.0 + nrm(ks[23], (d,), 0.02),
    }


def reference(x, rel_bias, ev_norm_mix, ev_w_in, ev_pool_w, ev_pool_scale, ev_conv_w, ev_w_out,
              ev_norm_ffn, ev_ffn_gate, ev_ffn_up, ev_ffn_down, od_norm_mix, od_w_qkv, od_b_qkv,
              od_sinks, od_w_o, od_b_o, od_norm_ffn, od_router, od_exp_gate, od_exp_up,
              od_exp_down, final_norm):
    for layer in range(DEPTH):
        i = layer // 2
        if layer % 2 == 0:
            h = rmsnorm(x, ev_norm_mix[i])
            x = x + pool_conv_mixer(h, ev_w_in[i], ev_pool_w[i], ev_pool_scale[i],
                                    ev_conv_w[i], ev_w_out[i])
            h = rmsnorm(x, ev_norm_ffn[i])
            x = x + swiglu(h, ev_ffn_gate[i], ev_ffn_up[i], ev_ffn_down[i])
        else:
            h = rmsnorm(x, od_norm_mix[i])
            x = x + sliding_window_attention(h, od_w_qkv[i], od_b_qkv[i], od_sinks[i],
                                             od_w_o[i], od_b_o[i], rel_bias)
            h = rmsnorm(x, od_norm_ffn[i])
            x = x + moe_swiglu(h, od_router[i], od_exp_gate[i], od_exp_up[i], od_exp_down[i])
    return rmsnorm(x, final_norm)
```

```python
import contextlib
import numpy as np
import concourse.bass as bass
import concourse.mybir as mybir
from concourse.bass_utils import run_bass_kernel_spmd

F32 = mybir.dt.float32
BF16 = mybir.dt.bfloat16
AF = mybir.ActivationFunctionType
ALU = mybir.AluOpType
AX = mybir.AxisListType

D = 1024
T = 1024
HALO = 256
TH = T + HALO
NB = TH // 128
TOK_CORE = 4096
NCORES = 8
SEQ = 16384
DFF0 = 2816
DFFE = 3584
NE = 8
NEG = -1.0e30
NSLOT = 6
POOL_W = (2, 4, 8, 16)

CFG = {"nst": 4, "stop": None, "ncores": 8, "nexp": 8, "sparse": True, "guard": True, "bpp": 4}

_BT_RLE = [[15, 101], [14, 27], [13, 18], [12, 14], [11, 9], [10, 7], [9, 4], [8, 4], [7, 1], [6, 1], [5, 1],
           [4, 1], [3, 1], [2, 1], [1, 1], [0, 1], [17, 1], [18, 1], [19, 1], [20, 1], [21, 1], [22, 1], [23, 1],
           [24, 4], [25, 4], [26, 7], [27, 9], [28, 14], [29, 18]]
_BT = [v for v, n in _BT_RLE for _ in range(n)]
assert len(_BT) == 255


class Eng:
    def __init__(self, name, eng, sem, selfwait=True):
        self.name = name
        self.eng = eng
        self.sem = sem
        self.count = 0
        self.seen = {}
        self.selfwait = selfwait

    def wait(self, ev):
        if ev is None:
            return
        sem, val, owner = ev
        if owner is self:
            if not self.selfwait:
                return
            if val < self.count - 1:
                return
        k = id(sem)
        if self.seen.get(k, 0) >= val:
            return
        self.eng.wait_ge(sem, val)
        self.seen[k] = val


def eng_need(E, ev, seen):
    if ev is None:
        return None
    sem, val, owner = ev
    if owner is E:
        if not E.selfwait:
            return None
        if val < E.count - 1:
            return None
    k = id(sem)
    if seen.get(k, 0) >= val:
        return None
    seen[k] = val
    return (sem, val)


class Tracker:
    def __init__(self):
        self.w = {}
        self.r = {}

    def deps(self, reads, writes):
        evs = []
        for k in reads:
            e = self.w.get(k)
            if e is not None:
                evs.append(e)
        for k in writes:
            e = self.w.get(k)
            if e is not None:
                evs.append(e)
            rr = self.r.get(k)
            if rr:
                evs.extend(rr.values())
        return evs

    def commit(self, ev, who, reads, writes):
        for k in reads:
            self.r.setdefault(k, {})[who] = ev
        for k in writes:
            self.w[k] = ev
            self.r[k] = {}


class DmaSem:
    def __init__(self, name, sem):
        self.name = name
        self.sem = sem
        self.count = 0


def build_nc(cfg):
    nst = cfg["nst"]
    stop = cfg["stop"]
    nexp = cfg["nexp"]
    nc = bass.Bass("TRN2", target_bir_lowering=False)

    def din(name, shape):
        return nc.dram_tensor(name, list(shape), F32, kind="ExternalInput").ap()

    x_d = din("x", [TOK_CORE + HALO, D])
    rel_bias_d = din("rel_bias", [32, 16])
    ev_norm_mix = din("ev_norm_mix", [1, D])
    ev_w_in = din("ev_w_in", [1, D, 2048])
    ev_pool_w = din("ev_pool_w", [1, 4, 128, 128])
    ev_pool_scale = din("ev_pool_scale", [1, 512])
    ev_conv_w = din("ev_conv_w", [1, 3, 512])
    ev_w_out = din("ev_w_out", [1, D, D])
    ev_norm_ffn = din("ev_norm_ffn", [1, D])
    ev_ffn_gate = din("ev_ffn_gate", [1, D, DFF0])
    ev_ffn_up = din("ev_ffn_up", [1, D, DFF0])
    ev_ffn_down = din("ev_ffn_down", [1, DFF0, D])
    od_norm_mix = din("od_norm_mix", [1, D])
    od_w_qkv = din("od_w_qkv", [1, D, 1280])
    od_b_qkv = din("od_b_qkv", [1, 1280])
    od_sinks = din("od_sinks", [1, 16])
    od_w_o = din("od_w_o", [1, D, D])
    od_b_o = din("od_b_o", [1, D])
    od_norm_ffn = din("od_norm_ffn", [1, D])
    od_router = din("od_router", [1, D, NE])
    od_exp_gate = din("od_exp_gate", [1, NE, D, DFFE])
    od_exp_up = din("od_exp_up", [1, NE, D, DFFE])
    od_exp_down = din("od_exp_down", [1, NE, DFFE, D])
    final_norm = din("final_norm", [D])
    c_oh = din("c_oh", [32, 383])
    c_kmask = din("c_kmask", [128, 4 * NB])
    c_corr = din("c_corr", [128, 4 * 4 * 16])
    c_ident = din("c_ident", [128, 128])
    c_vmask = din("c_vmask", [128, 4])
    c_ltri = din("c_ltri", [128, 128])
    c_iota = din("c_iota", [128, 128])
    c_iotacol = din("c_iotacol", [128, 8])
    y_d = nc.dram_tensor("y", [TOK_CORE, D], F32, kind="ExternalOutput").ap()
    wcache = nc.dram_tensor("wcache", [NE * 7 * 3, 128, 4096], BF16, kind="Internal").ap()

    es = contextlib.ExitStack()
    with es:
        def sb(name, shape, dt):
            return es.enter_context(nc.sbuf_tensor(name, list(shape), dt))

        def sem(name):
            return es.enter_context(nc.semaphore(name))

        x_sb = sb("x_sb", [128, NB, D], F32)
        hT = sb("hT", [128, 8, TH], BF16)
        ring = sb("ring", [128, NSLOT, 4096], BF16)
        xn = sb("xn", [128, 2, D], BF16)
        junk = sb("junk", [128, D], BF16)
        ob = sb("ob", [128, 2, D], F32)
        biasT = sb("biasT", [128, 4, 2, 512], F32)
        esink = sb("esink", [128, 2, 512], F32)
        gfin = sb("gfin", [128, D], F32)
        gfm = sb("gfm", [128, 4, 8], F32)
        ssq = sb("ssq", [128, 16], F32)
        rstd = sb("rstd", [128, 16], F32)
        identb = sb("identb", [128, 128], BF16)
        onesb = sb("onesb", [128, 128], BF16)
        bo_row = sb("bo_row", [1, D], BF16)
        bv_row = sb("bv_row", [1, 128], BF16)
        bqk = sb("bqk", [64, 18], F32)
        kmask = sb("kmask", [128, 4 * NB], F32)
        corr = sb("corr", [128, 256], F32)
        vmask = sb("vmask", [128, 4], F32)
        poolw = sb("poolw", [128, 4, 128], BF16)
        pscale = sb("pscale", [128, 4], F32)
        cw = sb("cw", [128, 3, 4], F32)
        wr = sb("wr", [128, 8, NE], BF16)
        oh = sb("oh", [32, 383], F32)
        relb = sb("relb", [32, 16], F32)
        es16 = sb("es16", [128, 16], F32)
        esr = sb("esr", [1, 2, 512], BF16)
        comb = sb("comb", [128, 8, NE], F32)
        rt = sb("rt", [128, 8, 8], F32)
        rs = sb("rs", [128, 16], F32)
        Dd = sb("Dd", [128, 2, 128], F32)
        iota_f = sb("iota_f", [128, 128], F32)
        identf = sb("identf", [128, 128], F32)
        onesf = sb("onesf", [128, 128], F32)
        ltri = sb("ltri", [128, 128], BF16)
        iotacol = sb("iotacol", [128, 8], F32)
        sel_f = sb("sel_f", [128, 64], F32)
        sel_bf = sb("sel_bf", [128, 64], BF16)
        pos_sb = sb("pos_sb", [128, 64], F32)
        posm = sb("posm", [128, 64], F32)
        pshift = sb("pshift", [128, 8], F32)
        n_f = sb("n_f", [1, 8], F32)
        n_i = sb("n_i", [1, 8], mybir.dt.int32)
        dum = sb("dum", [128, 4], F32)
        dumv = sb("dumv", [128, 4], F32)
        dumb = sb("dumb", [2, 2], BF16)
        dumd = sb("dumd", [1, 4], F32)

        UBYTES = 49152
        U = sb("U", [128, UBYTES // 2], BF16)

        def uview(off_b, nbytes, dt):
            a = U[:, off_b // 2:(off_b + nbytes) // 2]
            if dt == F32:
                a = a.bitcast(F32)
            return a

        PADW = 16 + TH
        yT = uview(0, 8 * TH * 2, BF16).rearrange("p (k t) -> p k t", k=8)
        mb = [uview(20480 + i * PADW * 4, PADW * 4, F32) for i in range(3)]
        pooled = uview(20480 + 3 * PADW * 4, TH * 2, BF16)
        TA = TH - 128
        aT = uview(0, 2 * 4 * TA * 2, BF16).rearrange("p (a c t) -> p a c t", a=2, c=4)
        sg = uview(18432, 2 * 512 * 4, F32).rearrange("p (a t) -> p a t", a=2)
        qT = uview(0, 8 * 512 * 2, BF16).rearrange("p (h t) -> p h t", h=8)
        kT = uview(8192, 2 * TH * 2, BF16).rearrange("p (g t) -> p g t", g=2)
        vv = uview(13312, NB * 256 * 2, BF16).rearrange("p (b g e) -> p b g e", b=NB, g=2)
        tbuf = uview(18432, 4 * 512 * 4, F32).rearrange("p (a t) -> p a t", a=4)
        pT = uview(26624, 4 * 512 * 2, BF16).rearrange("p (a t) -> p a t", a=4)
        rden = uview(30720, 512 * 4, F32)
        oT = uview(32768, 8 * T * 2, BF16).rearrange("p (k t) -> p k t", k=8)

        hn = uview(0, 8 * D * 2, BF16).rearrange("p (b d) -> p b d", b=8)
        hTe = uview(16384, 8 * 512 * 2, BF16).rearrange("p (k t) -> p k t", k=8)
        o_sl = uview(24576, 4 * D * 4, F32).rearrange("p (j d) -> p j d", j=4)
        a_sl = uview(40960, 2 * 512 * 2, BF16).rearrange("p (a t) -> p a t", a=2)
        sgm = uview(43008, 2 * 512 * 4, F32).rearrange("p (a t) -> p a t", a=2)
        aTe = uview(47104, 2 * 512 * 2, BF16).rearrange("p (a t) -> p a t", a=2)
        hTe2 = x_sb[:, 0:2, :].rearrange("p b d -> p (b d)").bitcast(BF16).rearrange("p (k t) -> p k t", k=8)
        hTe_bufs = [hTe, hTe2]
        XH = [("x", 0, 0), ("x", 0, 1), ("x", 1, 0), ("x", 1, 1)]
        hT_flat = hT[:].rearrange("p k t -> p (k t)")
        STj4 = hT_flat[:, 0:4096].rearrange("p (j t) -> p j t", j=4)
        o_bf4 = hT_flat[:, 4096:8192].rearrange("p (j t) -> p j t", j=4)
        HTK = [("hT", b_) for b_ in range(NB)]
        Sg = xn[:, 0, :].rearrange("p (i s) -> p i s", i=8)
        o_bf = xn[:, 1, :]
        STj = junk

        ps = es.enter_context(nc.psum_tensor("ps", [128, 6, 512], F32))
        psb = es.enter_context(nc.psum_tensor("psb", [128, 2, 1024], BF16))

        TR = Tracker()
        PE = Eng("pe", nc.tensor, sem("s_pe"), selfwait=False)
        ACT = Eng("act", nc.scalar, sem("s_act"))
        DVE = Eng("dve", nc.vector, sem("s_dve"))
        POOL = Eng("pool", nc.gpsimd, sem("s_pool"))
        SP = Eng("sp", nc.sync, sem("s_sp"))
        slot_sems = [DmaSem("slot%d" % i, sem("s_slot%d" % i)) for i in range(NSLOT)]
        xld = DmaSem("xld", sem("s_xld"))
        xlds = [DmaSem("xld%d" % i, sem("s_xld%d" % i)) for i in range(NB)]
        cst = DmaSem("cst", sem("s_cst"))
        cst2 = DmaSem("cst2", sem("s_cst2"))
        ost = [DmaSem("ost%d" % i, sem("s_ost%d" % i)) for i in range(2)]
        wb_sems = [DmaSem("wb%d" % i, sem("s_wb%d" % i)) for i in range(NSLOT)]
        all_dsems = slot_sems + [xld, cst, cst2] + ost

        REC = {"on": False, "items": {}, "seen": {}}

        def op(E, fn, reads=(), writes=(), force=False):
            if REC["on"] and not force:
                seen = REC["seen"].setdefault(E.name, dict(E.seen))
                waits = []
                for ev in TR.deps(reads, writes):
                    w = eng_need(E, ev, seen)
                    if w is not None:
                        waits.append(w)
                E.count += 1
                ev = (E.sem, E.count, E)
                REC["items"].setdefault(E.name, []).append((waits, fn, None))
                TR.commit(ev, E.name, reads, writes)
                return ev
            for ev in TR.deps(reads, writes):
                E.wait(ev)
            ins = fn()
            E.count += 1
            ins.then_inc(E.sem, 1)
            ev = (E.sem, E.count, E)
            TR.commit(ev, E.name, reads, writes)
            return ev

        def dma(E, ds, out, in_, reads=(), writes=(), **kw):
            if REC["on"]:
                seen = REC["seen"].setdefault(E.name, dict(E.seen))
                waits = []
                for ev in TR.deps(reads, writes):
                    w = eng_need(E, ev, seen)
                    if w is not None:
                        waits.append(w)
                ds.count += 16
                ev = (ds.sem, ds.count, ds)
                REC["items"].setdefault(E.name, []).append(
                    (waits, lambda: E.eng.dma_start(out=out, in_=in_, **kw), (ds, ds.count - 16)))
                TR.commit(ev, "dma:" + ds.name, reads, writes)
                return ev
            for ev in TR.deps(reads, writes):
                E.wait(ev)
            E.eng.dma_start(out=out, in_=in_, **kw).then_inc(ds.sem, 16)
            ds.count += 16
            ev = (ds.sem, ds.count, ds)
            TR.commit(ev, "dma:" + ds.name, reads, writes)
            return ev

        def barrier():
            engs = [PE, ACT, DVE]
            for E in engs:
                for Fo in engs:
                    if Fo is not E and Fo.count > 0:
                        E.wait((Fo.sem, Fo.count, Fo))

        ENG_BY_NAME = {"pe": PE, "act": ACT, "dve": DVE, "pool": POOL, "sp": SP}
        REGS = {}

        def get_reg(E, par=0):
            k_ = (E.name, par)
            if k_ not in REGS:
                REGS[k_] = es.enter_context(E.eng.register("r_%s%d" % (E.name, par)))
            return REGS[k_]

        def emit_dummy(E, n):
            if E is PE:
                ins = nc.tensor.transpose(psb[0:1, 1, 0:2], dumb[0:2, 0:1], identb[0:2, 0:2])
            elif E is ACT:
                ins = nc.scalar.activation(out=dum[:, 0:1], in_=dum[:, 1:2], func=AF.Silu)
            elif E is DVE:
                ins = nc.vector.memset(dumv[:, 2:3], 0.0)
            else:
                raise AssertionError(E.name)
            ins.then_inc(E.sem, n)

        @contextlib.contextmanager
        def task(thr, guard=True, par=0):
            if not guard:
                yield
                return
            assert not REC["on"]
            REC["on"] = True
            REC["items"] = {}
            REC["seen"] = {}
            try:
                yield
            finally:
                REC["on"] = False
            for name, items in REC["items"].items():
                E = ENG_BY_NAME[name]
                reg = get_reg(E, par)
                with E.eng.If_lt(reg, thr + 1):
                    if E is POOL or E is SP:
                        for (_, _, (ds, prev)) in items:
                            if prev > 0:
                                E.eng.wait_ge(ds.sem, prev)
                            E.eng.sem_inc(ds.sem, 16)
                    else:
                        emit_dummy(E, len(items))
                with E.eng.Else():
                    for (waits, fn, ds) in items:
                        for (sm, vl) in waits:
                            E.eng.wait_ge(sm, vl)
                        ins = fn()
                        if ds is None:
                            ins.then_inc(E.sem, 1)
                        else:
                            ins.then_inc(ds[0].sem, 16)

        def load_reg(E, ap, ev, par=0):
            E.wait(ev)
            E.eng.reg_load(get_reg(E, par), ap)

        rot = {"g": [0, 1], "u": [2, 3], "d": [4, 5], "a": [0, 1, 2, 3, 4, 5], "s": [0, 1, 2, 3]}
        rpos = {k: 0 for k in rot}

        def bank(kind):
            b = rot[kind][rpos[kind] % len(rot[kind])]
            rpos[kind] += 1
            return b

        ring_pos = [0]

        def col_slab(Wap, c0, ncols):
            s = ring_pos[0] % NSLOT
            ring_pos[0] += 1
            view = ring[:, s, 0:8 * ncols].rearrange("p (k n) -> p k n", k=8)
            dma(POOL, slot_sems[s], view, Wap[:, c0:c0 + ncols].rearrange("(k p) n -> p k n", p=128),
                writes=[("slab", s)])
            return view, ("slab", s)

        def row_slab(Wap, r0, nch):
            s = ring_pos[0] % NSLOT
            ring_pos[0] += 1
            view = ring[:, s, 0:nch * 1024].rearrange("p (c n) -> p c n", c=nch)
            dma(POOL, slot_sems[s], view, Wap[r0:r0 + nch * 128, :].rearrange("(c p) n -> p c n", p=128),
                writes=[("slab", s)])
            return view, ("slab", s)

        wb_pending = []

        def flush_wb():
            while wb_pending:
                idx_, key_ = wb_pending.pop(0)
                s_ = key_[1]
                dma(POOL, wb_sems[s_], wcache[idx_], ring[:, s_, :], reads=[key_], writes=[("wc", idx_)])

        def cache_slab(idx, is_col):
            s_ = ring_pos[0] % NSLOT
            ring_pos[0] += 1
            if is_col:
                view = ring[:, s_, :].rearrange("p (k n) -> p k n", k=8)
            else:
                view = ring[:, s_, :].rearrange("p (c n) -> p c n", c=4)
            dma(POOL, slot_sems[s_], ring[:, s_, :], wcache[idx], reads=[("wc", idx)], writes=[("slab", s_)])
            return view, ("slab", s_)

        def blocks_of(t0, tn):
            return list(range(t0 // 128, (t0 + tn + 127) // 128))

        def hkeys(t0, tn):
            return [("hT", b) for b in blocks_of(t0, tn)]

        def cload(out, in_, key, eng=None, ds=None, **kw):
            dma(eng or SP, ds or cst, out, in_, writes=[key], **kw)

        norm_vecs = [ev_norm_mix[0], ev_norm_ffn[0], od_norm_mix[0], od_norm_ffn[0]]
        for i, v in enumerate(norm_vecs):
            cload(gfm[:, i, :], v.rearrange("(k p) -> p k", p=128), "gfm", allow_slow_non_contiguous=True)
        cload(gfin[:], final_norm.partition_broadcast(128), "gfin")
        cload(identb[:], c_ident, "identb", eng=POOL, ds=cst2)
        cload(bo_row[:], od_b_o, "bo_row", eng=POOL, ds=cst2)
        cload(bv_row[:], od_b_qkv[:, 1152:1280], "bv_row", eng=POOL, ds=cst2)
        cload(bqk[:], od_b_qkv[0, 0:1152].rearrange("(h d) -> d h", d=64), "bqk", allow_slow_non_contiguous=True)
        cload(kmask[:], c_kmask, "kmask")
        cload(corr[:], c_corr, "corr")
        cload(vmask[:], c_vmask, "vmask")
        cload(poolw[:], ev_pool_w[0].rearrange("g c d -> c g d"), "poolw", eng=POOL, ds=cst2)
        cload(pscale[:], ev_pool_scale[0].rearrange("(g p) -> p g", p=128), "pscale", allow_slow_non_contiguous=True)
        cload(cw[:], ev_conv_w[0].rearrange("k (j p) -> p k j", p=128), "cw", allow_slow_non_contiguous=True)
        cload(wr[:], od_router[0].rearrange("(k p) e -> p k e", p=128), "wr", eng=POOL, ds=cst2)
        cload(oh[:], c_oh, "oh")
        cload(relb[:], rel_bias_d, "relb")
        cload(es16[:], od_sinks[0].partition_broadcast(128), "es16")
        cload(iota_f[:], c_iota, "iota_f")
        cload(identf[:], c_ident, "identf")
        cload(iotacol[:], c_iotacol, "iotacol")
        cload(ltri[:], c_ltri, "ltri", eng=POOL, ds=cst2)

        for k_ in list(TR.w.keys()):
            ev_ = TR.w[k_]
            if ev_[2] is cst:
                TR.w[k_] = (cst.sem, cst.count, cst)
            elif ev_[2] is cst2:
                TR.w[k_] = (cst2.sem, cst2.count, cst2)
        op(DVE, lambda: nc.vector.memset(onesb[:], 1.0), writes=["onesb"])
        op(DVE, lambda: nc.vector.memset(onesf[:], 1.0), writes=["onesf"])
        ev_da = op(DVE, lambda: nc.vector.memset(dum[:], 0.0), writes=["dum"])
        op(DVE, lambda: nc.vector.memset(dumv[:], 0.0), writes=["dumv"])
        ev_db = op(DVE, lambda: nc.vector.memset(dumb[:], 0.0), writes=["dumb"])
        ACT.wait(ev_da)
        PE.wait(ev_db)
        for i in range(3):
            op(DVE, lambda i=i: nc.vector.memset(mb[i][:, 0:16], 0.0), writes=[("mbpad", i)])

        op(ACT, lambda: nc.scalar.activation(out=es16[:], in_=es16[:], func=AF.Exp), reads=["es16"], writes=["es16"])
        for g in range(2):
            op(DVE, lambda g=g: nc.vector.tensor_copy(
                out=esink[:, g, :].rearrange("p (h q) -> p h q", h=8),
                in_=es16[:, g * 8:(g + 1) * 8].unsqueeze(2).to_broadcast([128, 8, 64])),
               reads=["es16"], writes=["esink"])

        for g in range(2):
            op(DVE, lambda g=g: nc.vector.tensor_copy(
                out=esr[0:1, g, :].rearrange("p (h q) -> p h q", h=8),
                in_=es16[0:1, g * 8:(g + 1) * 8].unsqueeze(2).to_broadcast([1, 8, 64])),
               reads=["es16"], writes=["esr"])
        offs = [-128, 0, -192, -64]
        for v in range(4):
            b0 = bank("a")
            b1 = bank("a")
            assert b1 == b0 + 1

            def mm_bias(v=v, b0=b0):
                ins = None
                for q in range(64):
                    s0 = 255 + offs[v] - q
                    dst = ps[:, b0 + q // 32, (q % 32) * 16:(q % 32) * 16 + 16]
                    ins = nc.tensor.matmul(dst, oh[:, s0:s0 + 128], relb[:], start=True, stop=True)
                return ins
            op(PE, mm_bias, reads=["oh", "relb"], writes=[("ps", b0), ("ps", b1)])
            for g in range(2):
                def cp(v=v, g=g, b0=b0):
                    src = ps[:, b0:b0 + 2, :].rearrange("p a (q h) -> p (a q) h", h=16)[:, :, g * 8:(g + 1) * 8]
                    return nc.vector.tensor_scalar(
                        out=biasT[:, v, g, :].rearrange("p (h q) -> p h q", h=8),
                        in0=src.rearrange("p q h -> p h q"),
                        scalar1=vmask[:, v:v + 1], scalar2=None, op0=ALU.add)
                op(DVE, cp, reads=[("ps", b0), ("ps", b0 + 1), "vmask"], writes=["biasT"])

        def rmsnorm_to_hT(ni, blks, keep=False):
            for b in blks:
                op(ACT, lambda b=b: nc.scalar.activation(out=junk[:], in_=x_sb[:, b, :], func=AF.Square,
                                                         accum_out=ssq[:, b:b + 1]),
                   reads=[("x", b, 0), ("x", b, 1)], writes=["STj", ("ssq", b)])
            b0, b1 = blks[0], blks[-1] + 1
            sk = [("ssq", b) for b in blks]
            rk = [("rstd", b) for b in blks]
            op(DVE, lambda: nc.vector.tensor_scalar(out=rstd[:, b0:b1], in0=ssq[:, b0:b1], scalar1=1.0 / D,
                                                    scalar2=1e-6, op0=ALU.mult, op1=ALU.add),
               reads=sk, writes=rk)
            op(ACT, lambda: nc.scalar.activation(out=rstd[:, b0:b1], in_=rstd[:, b0:b1], func=AF.Sqrt),
               reads=rk, writes=rk)
            op(DVE, lambda: nc.vector.reciprocal(out=rstd[:, b0:b1], in_=rstd[:, b0:b1]), reads=rk, writes=rk)
            for i, b in enumerate(blks):
                j = i % 2
                if keep:
                    xsrc = hn[:, b - 2, :]
                    xkey = ("hn", b - 2)
                else:
                    xsrc = xn[:, j, :]
                    xkey = ("xn", j)
                if i % 2 == 0:
                    op(ACT, lambda b=b, xsrc=xsrc: nc.scalar.activation(out=xsrc, in_=x_sb[:, b, :], func=AF.Copy,
                                                                        scale=rstd[:, b:b + 1]),
                       reads=[("x", b, 0), ("x", b, 1), ("rstd", b)], writes=[xkey])
                else:
                    op(DVE, lambda b=b, xsrc=xsrc: nc.vector.tensor_scalar(out=xsrc, in0=x_sb[:, b, :],
                                                                           scalar1=rstd[:, b:b + 1], scalar2=None,
                                                                           op0=ALU.mult),
                       reads=[("x", b, 0), ("x", b, 1), ("rstd", b)], writes=[xkey])

                def tr(xsrc=xsrc):
                    ins = None
                    for k in range(8):
                        ins = nc.tensor.transpose(psb[:, 0, k * 128:(k + 1) * 128], xsrc[:, k * 128:(k + 1) * 128],
                                                  identb[:])
                    return ins
                op(PE, tr, reads=[xkey, "identb"], writes=[("psb", 0)])
                op(DVE, lambda b=b: nc.vector.tensor_tensor(
                    out=hT[:, :, b * 128:(b + 1) * 128],
                    in0=psb[:, 0, :].rearrange("p (k t) -> p k t", k=8),
                    in1=gfm[:, ni, :].unsqueeze(2).to_broadcast([128, 8, 128]), op=ALU.mult),
                   reads=[("psb", 0), "gfm"], writes=[("hT", b)])

        def proj_fm(slab, skey, col0, t0, tn, M=128, bk=None):
            b = bank("a") if bk is None else bk

            def mm():
                ins = None
                for k in range(8):
                    ins = nc.tensor.matmul(ps[0:M, b, 0:tn], slab[:, k, col0:col0 + M], hT[:, k, t0:t0 + tn],
                                           start=(k == 0), stop=(k == 7))
                return ins
            op(PE, mm, reads=[skey] + hkeys(t0, tn), writes=[("ps", b)])
            return b

        def down_proj(A_fn, akeys_fn, slabs, nch, blks, tok_off, comb_fn=None, bias_row=None):
            for b in blks:
                t0 = b * 128 - tok_off
                for dh in range(2):
                    bk = bank("d")
                    rk = list(akeys_fn(b))
                    for (_, sk, _) in slabs:
                        rk.append(sk)
                    if bias_row is not None:
                        rk.append("bo_row")
                        rk.append("onesb")

                    def mm(b=b, dh=dh, bk=bk, t0=t0):
                        ins = None
                        first = True
                        if bias_row is not None:
                            ins = nc.tensor.matmul(ps[:, bk, :], onesb[0:1, :], bias_row[0:1, dh * 512:(dh + 1) * 512],
                                                   start=True, stop=False)
                            first = False
                        c = 0
                        for (sv, _, ns) in slabs:
                            for cc in range(ns):
                                ins = nc.tensor.matmul(ps[:, bk, :], A_fn(c, t0), sv[:, cc, dh * 512:(dh + 1) * 512],
                                                       start=first, stop=(c == nch - 1))
                                first = False
                                c += 1
                        return ins
                    op(PE, mm, reads=rk, writes=[("ps", bk)])
                    xs = x_sb[:, b, dh * 512:(dh + 1) * 512]
                    if comb_fn is None:
                        op(DVE, lambda bk=bk, xs=xs: nc.vector.tensor_tensor(out=xs, in0=ps[:, bk, :], in1=xs,
                                                                             op=ALU.add),
                           reads=[("ps", bk), ("x", b, dh)], writes=[("x", b, dh)])
                    else:
                        cap, ckey = comb_fn(b)
                        op(DVE, lambda bk=bk, xs=xs, cap=cap: nc.vector.scalar_tensor_tensor(
                            out=xs, in0=ps[:, bk, :], scalar=cap, in1=xs, op0=ALU.mult, op1=ALU.add),
                           reads=[("ps", bk), ("x", b, dh), ckey], writes=[("x", b, dh)])

        def ffn(Wg, Wu, Wd, nch, tiles, blks, comb_fn):
            units = [(c0, min(4, nch - c0)) for c0 in range(0, nch, 4)]
            pend = None
            sgi = [0]

            def emit_down(pd):
                dsv, dsk, ab, ncu = pd

                def A_fn(c, t0, ab=ab):
                    return aT[:, ab, c, t0 - 128:t0 - 128 + 128]

                def akeys(b, ab=ab, ncu=ncu):
                    ti = None
                    for i, (t0, tn) in enumerate(tiles):
                        if t0 <= b * 128 < t0 + tn:
                            ti = i
                    return [("aT", ab, c, ti) for c in range(ncu)]
                down_proj(A_fn, akeys, [(dsv, dsk, ncu)], ncu, blks, 0, comb_fn=comb_fn)

            for ui, (c0, ncu) in enumerate(units):
                gsv, gsk = col_slab(Wg, c0 * 128, ncu * 128)
                usv, usk = col_slab(Wu, c0 * 128, ncu * 128)
                dsv, dsk = row_slab(Wd, c0 * 128, ncu)
                ab = ui % 2
                for cc in range(ncu):
                    for ti, (t0, tn) in enumerate(tiles):
                        bg = bank("g")
                        bu = bank("u")
                        proj_fm(gsv, gsk, cc * 128, t0, tn, bk=bg)
                        proj_fm(usv, usk, cc * 128, t0, tn, bk=bu)
                        j = sgi[0] % 2
                        sgi[0] += 1
                        op(ACT, lambda bg=bg, j=j, tn=tn: nc.scalar.activation(out=sg[:, j, 0:tn], in_=ps[:, bg, 0:tn],
                                                                               func=AF.Silu),
                           reads=[("ps", bg)], writes=[("sg", j)])
                        op(DVE, lambda bu=bu, j=j, tn=tn, t0=t0, ab=ab, cc=cc: nc.vector.tensor_tensor(
                            out=aT[:, ab, cc, t0 - 128:t0 - 128 + tn], in0=ps[:, bu, 0:tn], in1=sg[:, j, 0:tn],
                            op=ALU.mult),
                           reads=[("ps", bu), ("sg", j)], writes=[("aT", ab, cc, ti)])
                if pend is not None:
                    emit_down(pend)
                pend = (dsv, dsk, ab, ncu)
            emit_down(pend)

        def xkeys(blks):
            return [("x", b, dh) for b in blks for dh in range(2)]

        TT_ALL = [(0, 512), (512, 512), (1024, 256)]
        TT_19 = [(128, 384), (512, 384), (896, 384)]
        TT_29 = [(256, 512), (768, 512)]
        B_ALL = list(range(NB))
        B_19 = list(range(1, NB))
        B_29 = list(range(2, NB))
        out_i = [0]

        def dump_x(st):
            for b in B_29:
                j = out_i[0] % 2
                out_i[0] += 1
                op(DVE, lambda b=b, j=j: nc.vector.tensor_copy(out=ob[:, j, :], in_=x_sb[:, b, :]),
                   reads=xkeys([b]), writes=[("ob", j)])
                r0 = st * T + (b - 2) * 128
                dma(SP, ost[j], y_d[r0:r0 + 128, :], ob[:, j, :], reads=[("ob", j)])

        for st in range(nst):
            for b_ in B_ALL:
                dma(SP, xlds[b_], x_sb[:, b_, :], x_d[st * T + b_ * 128:st * T + (b_ + 1) * 128, :],
                    writes=xkeys([b_]))

            rmsnorm_to_hT(0, B_ALL)
            barrier()
            wslabs = [col_slab(ev_w_in[0], i * 512, 512) for i in range(4)]
            for g in range(4):
                for (t0, tn) in TT_ALL:
                    bk = proj_fm(wslabs[0][0], wslabs[0][1], g * 128, t0, tn)
                    op(ACT, lambda bk=bk, t0=t0, tn=tn: nc.scalar.copy(out=mb[0][:, 16 + t0:16 + t0 + tn],
                                                                       in_=ps[:, bk, 0:tn]),
                       reads=[("ps", bk)], writes=[("mb", 0)])
                src = 0
                dst = 1
                sh = 1
                for lvl in range(g + 1):
                    op(DVE, lambda src=src, dst=dst, sh=sh: nc.vector.tensor_tensor(
                        out=mb[dst][:, 16:16 + TH], in0=mb[src][:, 16:16 + TH], in1=mb[src][:, 16 - sh:16 - sh + TH],
                        op=ALU.add),
                       reads=[("mb", src), ("mbpad", src)], writes=[("mb", dst)])
                    src = dst
                    dst = 2 if dst == 1 else 1
                    sh *= 2
                op(DVE, lambda src=src, g=g, st=st: nc.vector.tensor_tensor(
                    out=mb[src][:, 16 + HALO:16 + HALO + 16], in0=mb[src][:, 16 + HALO:16 + HALO + 16],
                    in1=corr[:, (st * 4 + g) * 16:(st * 4 + g) * 16 + 16], op=ALU.mult),
                   reads=[("mb", src), "corr"], writes=[("mb", src)])
                op(DVE, lambda src=src, g=g: nc.vector.scalar_tensor_tensor(
                    out=pooled[:, :], in0=mb[src][:, 16:16 + TH], scalar=1.0 / POOL_W[g], in1=mb[0][:, 16:16 + TH],
                    op0=ALU.mult, op1=ALU.subtract),
                   reads=[("mb", src), ("mb", 0)], writes=["pooled"])
                for (t0, tn) in TT_ALL:
                    bk = bank("a")
                    op(PE, lambda bk=bk, t0=t0, tn=tn, g=g: nc.tensor.matmul(
                        ps[:, bk, 0:tn], poolw[:, g, :], pooled[:, t0:t0 + tn], start=True, stop=True),
                       reads=["poolw", "pooled"], writes=[("ps", bk)])
                    op(ACT, lambda bk=bk, t0=t0, tn=tn, g=g: nc.scalar.activation(
                        out=yT[:, g, t0:t0 + tn], in_=ps[:, bk, 0:tn], func=AF.Copy, scale=pscale[:, g:g + 1]),
                       reads=[("ps", bk), "pscale"], writes=[("yT", g)])
            for j in range(4):
                for (t0, tn) in TT_ALL:
                    bk = proj_fm(wslabs[2][0], wslabs[2][1], j * 128, t0, tn)
                    op(ACT, lambda bk=bk, t0=t0, tn=tn: nc.scalar.copy(out=mb[0][:, 16 + t0:16 + t0 + tn],
                                                                       in_=ps[:, bk, 0:tn]),
                       reads=[("ps", bk)], writes=[("mb", 0)])
                for (t0, tn) in TT_ALL:
                    bk = proj_fm(wslabs[3][0], wslabs[3][1], j * 128, t0, tn)
                    op(DVE, lambda bk=bk, t0=t0, tn=tn: nc.vector.tensor_tensor(
                        out=mb[1][:, 16 + t0:16 + t0 + tn], in0=ps[:, bk, 0:tn], in1=mb[0][:, 16 + t0:16 + t0 + tn],
                        op=ALU.mult),
                       reads=[("ps", bk), ("mb", 0)], writes=[("mb", 1)])
                op(DVE, lambda j=j: nc.vector.tensor_scalar(out=mb[2][:, 16:16 + TH], in0=mb[1][:, 16:16 + TH],
                                                            scalar1=cw[:, 2, j:j + 1], scalar2=None, op0=ALU.mult),
                   reads=[("mb", 1), "cw"], writes=[("mb", 2)])
                for kk in (1, 0):
                    shf = 2 - kk
                    op(DVE, lambda j=j, kk=kk, shf=shf: nc.vector.scalar_tensor_tensor(
                        out=mb[2][:, 16:16 + TH], in0=mb[1][:, 16 - shf:16 - shf + TH], scalar=cw[:, kk, j:j + 1],
                        in1=mb[2][:, 16:16 + TH], op0=ALU.mult, op1=ALU.add),
                       reads=[("mb", 1), ("mbpad", 1), ("mb", 2), "cw"], writes=[("mb", 2)])
                for (t0, tn) in TT_ALL:
                    bk = proj_fm(wslabs[1][0], wslabs[1][1], j * 128, t0, tn)
                    op(DVE, lambda bk=bk, t0=t0, tn=tn, j=j: nc.vector.tensor_tensor(
                        out=yT[:, 4 + j, t0:t0 + tn], in0=ps[:, bk, 0:tn], in1=mb[2][:, 16 + t0:16 + t0 + tn],
                        op=ALU.mult),
                       reads=[("ps", bk), ("mb", 2)], writes=[("yT", 4 + j)])
            wo = [row_slab(ev_w_out[0], i * 512, 4) for i in range(2)]
            down_proj(lambda c, t0: yT[:, c, t0:t0 + 128], lambda b: [("yT", c) for c in range(8)],
                      [(wo[0][0], wo[0][1], 4), (wo[1][0], wo[1][1], 4)], 8, B_19, 0)
            if stop == "mixer":
                dump_x(st)
                continue

            rmsnorm_to_hT(1, B_19)
            barrier()
            ffn(ev_ffn_gate[0], ev_ffn_up[0], ev_ffn_down[0], DFF0 // 128, TT_19, B_19, None)
            if stop == "ffn0":
                dump_x(st)
                continue

            rmsnorm_to_hT(2, B_19)
            barrier()
            wq = [col_slab(od_w_qkv[0], 0, 512), col_slab(od_w_qkv[0], 512, 512), col_slab(od_w_qkv[0], 1024, 256)]
            for g in range(2):
                for (t0, tn) in TT_19:
                    bk = proj_fm(wq[2][0], wq[2][1], g * 64, t0, tn, M=64)
                    op(ACT, lambda bk=bk, g=g, t0=t0, tn=tn: nc.scalar.activation(
                        out=kT[0:64, g, t0:t0 + tn], in_=ps[0:64, bk, 0:tn], func=AF.Identity,
                        bias=bqk[:, 16 + g:17 + g]),
                       reads=[("ps", bk), "bqk"], writes=[("kT", g)])
            for b in B_19:
                bk = bank("a")

                def mmv(b=b, bk=bk):
                    nc.tensor.matmul(ps[:, bk, 0:128], onesb[0:1, :], bv_row[0:1, :], start=True, stop=False)
                    ins = None
                    for k in range(8):
                        ins = nc.tensor.matmul(ps[:, bk, 0:128], hT[:, k, b * 128:(b + 1) * 128],
                                               wq[2][0][:, k, 128:256], start=False, stop=(k == 7))
                    return ins
                op(PE, mmv, reads=[wq[2][1], ("hT", b), "onesb", "bv_row"], writes=[("ps", bk)])
                op(DVE, lambda b=b, bk=bk: nc.vector.tensor_copy(
                    out=vv[:, b, :, :].rearrange("p g (e d) -> p g e d", e=2),
                    in_=ps[:, bk, 0:128].rearrange("p (g d) -> p g d", g=2).unsqueeze(2).to_broadcast([128, 2, 2, 64])),
                   reads=[("ps", bk)], writes=[("vv", b)])
            for sub in range(2):
                tq0 = HALO + sub * 512
                for g in range(2):
                    for h8 in range(8):
                        h = g * 8 + h8
                        bk = proj_fm(wq[h // 8][0], wq[h // 8][1], (h % 8) * 64, tq0, 512, M=64)
                        op(ACT, lambda bk=bk, h=h, h8=h8: nc.scalar.activation(
                            out=qT[0:64, h8, :], in_=ps[0:64, bk, :], func=AF.Identity, bias=bqk[:, h:h + 1]),
                           reads=[("ps", bk), "bqk"], writes=[("qT", h8)])
                    def att_s1(ci, g=g, tq0=tq0, sub=sub):
                        q0 = tq0 + ci * 64
                        odd = (q0 // 64) % 2
                        blkA = (q0 - 128) // 128
                        blkB = blkA + 1
                        var = (2, 3) if odd else (0, 1)
                        sb_ = []
                        for ii, blk in enumerate((blkA, blkB)):
                            bk = bank("a")
                            sb_.append(bk)
                            op(PE, lambda bk=bk, blk=blk, g=g, ci=ci: nc.tensor.matmul(
                                ps[:, bk, :], kT[0:64, g, blk * 128:(blk + 1) * 128],
                                qT[0:64, :, ci * 64:(ci + 1) * 64], start=True, stop=True),
                               reads=[("kT", g)] + [("qT", i) for i in range(8)], writes=[("ps", bk)])
                        tj = [(2 * ci + ii) % 4 for ii in range(2)]
                        for ii, blk in enumerate((blkA, blkB)):
                            op(DVE, lambda ii=ii, bk=sb_[ii], v=var[ii], g=g, j=tj[ii]: nc.vector.scalar_tensor_tensor(
                                out=tbuf[:, j, :], in0=ps[:, bk, :], scalar=0.125, in1=biasT[:, v, g, :],
                                op0=ALU.mult, op1=ALU.add),
                               reads=[("ps", sb_[ii]), "biasT"], writes=[("tbuf", tj[ii])])
                            op(ACT, lambda ii=ii, blk=blk, j=tj[ii], st=st: nc.scalar.activation(
                                out=pT[:, j, :], in_=tbuf[:, j, :], func=AF.Exp,
                                bias=kmask[:, st * NB + blk:st * NB + blk + 1]),
                               reads=[("tbuf", tj[ii]), "kmask"], writes=[("pT", tj[ii])])
                        return (ci, blkA, blkB, tj)

                    def att_s2(state, g=g, sub=sub):
                        ci, blkA, blkB, tj = state
                        bo = bank("a")
                        bd = bank("a")

                        def mmo(bo=bo, blkA=blkA, blkB=blkB, g=g, tj=tj):
                            nc.tensor.matmul(ps[:, bo, :], vv[:, blkA, g, :], pT[:, tj[0], :], start=True, stop=False)
                            return nc.tensor.matmul(ps[:, bo, :], vv[:, blkB, g, :], pT[:, tj[1], :], start=False,
                                                    stop=True)
                        op(PE, mmo, reads=[("vv", blkA), ("vv", blkB), ("pT", tj[0]), ("pT", tj[1])],
                           writes=[("ps", bo)])

                        def mmd(bd=bd, tj=tj, g=g):
                            nc.tensor.matmul(ps[:, bd, :], onesb[0:1, :], esr[0:1, g, :], start=True, stop=False)
                            nc.tensor.matmul(ps[:, bd, :], onesb[:, :], pT[:, tj[0], :], start=False, stop=False)
                            return nc.tensor.matmul(ps[:, bd, :], onesb[:, :], pT[:, tj[1], :], start=False, stop=True)
                        op(PE, mmd, reads=["onesb", "esr", ("pT", tj[0]), ("pT", tj[1])], writes=[("ps", bd)])
                        op(ACT, lambda bd=bd: nc.scalar.activation(out=rden[:, :], in_=ps[:, bd, :], func=AF.Ln),
                           reads=[("ps", bd)], writes=["rden"])
                        op(ACT, lambda: nc.scalar.activation(out=rden[:, :], in_=rden[:, :], func=AF.Exp, scale=-1.0),
                           reads=["rden"], writes=["rden"])
                        tok0 = sub * 512 + ci * 64
                        for half in range(2):
                            pr = slice(half * 64, half * 64 + 64)
                            op(DVE, lambda half=half, pr=pr, bo=bo, g=g, tok0=tok0: nc.vector.tensor_tensor(
                                out=oT[pr, g * 4:(g + 1) * 4, tok0:tok0 + 64],
                                in0=ps[pr, bo, :].rearrange("p (a e q) -> p a e q", a=4, e=2)[:, :, half, :],
                                in1=rden[pr, :].rearrange("p (a e q) -> p a e q", a=4, e=2)[:, :, half, :],
                                op=ALU.mult),
                               reads=[("ps", bo), "rden"], writes=[("oT", g, tok0 // 128)])

                    states = {}
                    for step in range(9):
                        if step < 8:
                            states[step] = att_s1(step)
                        if step >= 1:
                            att_s2(states[step - 1])

            wos = [row_slab(od_w_o[0], i * 512, 4) for i in range(2)]
            down_proj(lambda c, t0: oT[:, c, t0:t0 + 128],
                      lambda b: [("oT", g, b - 2) for g in range(2)],
                      [(wos[0][0], wos[0][1], 4), (wos[1][0], wos[1][1], 4)], 8, B_29, HALO, bias_row=bo_row)
            if stop == "attn":
                dump_x(st)
                continue

            sparse = cfg.get("sparse", True)
            rmsnorm_to_hT(3, B_29, keep=sparse)
            barrier()
            for b in B_29:
                i = b - 2
                bk = bank("a")

                def mmr(b=b, bk=bk):
                    ins = None
                    for k in range(8):
                        ins = nc.tensor.matmul(ps[:, bk, 0:NE], hT[:, k, b * 128:(b + 1) * 128], wr[:, k, :],
                                               start=(k == 0), stop=(k == 7))
                    return ins
                op(PE, mmr, reads=[("hT", b), "wr"], writes=[("ps", bk)])
                lg = rt[:, 0, :]
                e1 = rt[:, 1, :]
                l2 = rt[:, 2, :]
                sel = rt[:, 3, :]
                ex = rt[:, 4, :]
                m1 = rs[:, 0:1]
                m2 = rs[:, 1:2]
                nm1 = rs[:, 2:3]
                den = rs[:, 3:4]
                K = "rt"
                op(DVE, lambda bk=bk: nc.vector.tensor_copy(out=lg, in_=ps[:, bk, 0:NE]), reads=[("ps", bk)], writes=[K])
                op(DVE, lambda: nc.vector.reduce_max(out=m1, in_=lg, axis=AX.X), reads=[K], writes=[K])
                op(DVE, lambda: nc.vector.tensor_scalar(out=e1, in0=lg, scalar1=m1, scalar2=NEG, op0=ALU.is_equal,
                                                        op1=ALU.mult), reads=[K], writes=[K])
                op(DVE, lambda: nc.vector.tensor_tensor(out=l2, in0=e1, in1=lg, op=ALU.add), reads=[K], writes=[K])
                op(DVE, lambda: nc.vector.reduce_max(out=m2, in_=l2, axis=AX.X), reads=[K], writes=[K])
                op(DVE, lambda: nc.vector.tensor_scalar(out=sel, in0=lg, scalar1=m2, scalar2=None, op0=ALU.is_ge),
                   reads=[K], writes=[K])
                op(DVE, lambda: nc.vector.tensor_scalar(out=nm1, in0=m1, scalar1=-1.0, scalar2=None, op0=ALU.mult),
                   reads=[K], writes=[K])
                op(ACT, lambda: nc.scalar.activation(out=ex, in_=lg, func=AF.Exp, bias=nm1), reads=[K], writes=[K])
                op(DVE, lambda: nc.vector.tensor_tensor(out=ex, in0=ex, in1=sel, op=ALU.mult), reads=[K], writes=[K])
                op(DVE, lambda: nc.vector.reduce_sum(out=den, in_=ex, axis=AX.X), reads=[K], writes=[K])
                op(DVE, lambda: nc.vector.reciprocal(out=den, in_=den), reads=[K], writes=[K])
                op(DVE, lambda i=i: nc.vector.tensor_scalar(out=comb[:, i, :], in0=ex, scalar1=den, scalar2=None,
                                                            op0=ALU.mult), reads=[K], writes=[("comb", i)])
                if sparse:
                    op(DVE, lambda i=i: nc.vector.tensor_copy(out=sel_f[:, i * 8:(i + 1) * 8], in_=sel), reads=[K],
                       writes=[("sel_f", i)])
                    op(DVE, lambda i=i: nc.vector.tensor_copy(out=sel_bf[:, i * 8:(i + 1) * 8], in_=sel), reads=[K],
                       writes=[("sel_bf", i)])
            if not sparse:
                for e in range(nexp):
                    ffn(od_exp_gate[0, e], od_exp_up[0, e], od_exp_down[0, e], DFFE // 128, TT_29, B_29,
                        lambda b, e=e: (comb[:, b - 2, e:e + 1], ("comb", b - 2)))
            else:
                guard = cfg.get("guard", True)
                BPP = cfg.get("bpp", 4)
                NPASS = 8 // BPP
                bkp = bank("a")
                bkn = bank("a")
                selk = [("sel_bf", i) for i in range(8)]

                def mmpos(bkp=bkp):
                    ins = None
                    for i in range(8):
                        ins = nc.tensor.matmul(ps[:, bkp, i * 8:(i + 1) * 8], ltri[:], sel_bf[:, i * 8:(i + 1) * 8],
                                               start=True, stop=(i == 0))
                        for i2 in range(i):
                            ins = nc.tensor.matmul(ps[:, bkp, i * 8:(i + 1) * 8], onesb[:], sel_bf[:, i2 * 8:(i2 + 1) * 8],
                                                   start=False, stop=(i2 == i - 1))
                    return ins
                op(PE, mmpos, reads=selk + ["ltri", "onesb"], writes=[("ps", bkp)])

                def mmn(bkn=bkn):
                    ins = None
                    for i in range(8):
                        ins = nc.tensor.matmul(ps[0:1, bkn, 0:8], onesb[:, 0:1], sel_bf[:, i * 8:(i + 1) * 8],
                                               start=(i == 0), stop=(i == 7))
                    return ins
                op(PE, mmn, reads=selk + ["onesb"], writes=[("ps", bkn)])
                op(DVE, lambda: nc.vector.tensor_scalar(out=pos_sb[:], in0=ps[:, bkp, 0:64], scalar1=1.0, scalar2=None,
                                                        op0=ALU.add), reads=[("ps", bkp)], writes=["pos_sb"])
                op(DVE, lambda: nc.vector.tensor_tensor(out=pos_sb[:], in0=pos_sb[:], in1=sel_f[:], op=ALU.mult),
                   reads=["pos_sb"] + [("sel_f", i) for i in range(8)], writes=["pos_sb"])
                op(DVE, lambda: nc.vector.tensor_scalar(out=posm[:], in0=pos_sb[:], scalar1=-1.0, scalar2=None,
                                                        op0=ALU.add), reads=["pos_sb"], writes=["posm"])
                op(DVE, lambda: nc.vector.tensor_copy(out=n_f[:], in_=ps[0:1, bkn, 0:8]), reads=[("ps", bkn)],
                   writes=["n_f"])
                ev_n = op(DVE, lambda: nc.vector.tensor_copy(out=n_i[:], in_=n_f[:]), reads=["n_f"], writes=["n_i"])
                posm_v = posm[:].rearrange("p (i e) -> p i e", e=8)
                units = [(c0, min(4, 28 - c0)) for c0 in range(0, 28, 4)]

                def gather_block(e, jj, j, par, g_):
                    hb = hTe_bufs[par]
                    xa = XH if par == 1 else []
                    with task(128 * jj, g_, par):
                        op(DVE, lambda e=e, jj=jj: nc.vector.tensor_scalar(
                            out=pshift[:], in0=posm_v[:, :, e], scalar1=-128.0 * jj, scalar2=None, op0=ALU.add),
                           reads=["posm"], writes=["pshift"])
                        i0 = min(jj, 7)
                        for i in range(i0, 8):
                            op(DVE, lambda i=i: nc.vector.tensor_scalar(
                                out=Sg[:, i, :], in0=iota_f[:], scalar1=pshift[:, i:i + 1], scalar2=None,
                                op0=ALU.is_equal),
                               reads=["pshift", "iota_f"], writes=[("Sg", i), ("xn", 0)])
                        for half in range(2):
                            bk = bank("s")

                            def mmg(half=half, bk=bk, i0=i0):
                                ins = None
                                for kk in range(4):
                                    k = half * 4 + kk
                                    for i in range(i0, 8):
                                        ins = nc.tensor.matmul(ps[:, bk, kk * 128:(kk + 1) * 128],
                                                               hn[:, i, k * 128:(k + 1) * 128], Sg[:, i, :],
                                                               start=(i == i0), stop=(i == 7))
                                return ins
                            op(PE, mmg, reads=[("hn", i) for i in range(8)] + [("Sg", i) for i in range(i0, 8)] + [("xn", 0)],
                               writes=[("ps", bk)])
                            op(DVE, lambda half=half, bk=bk, j=j, hb=hb: nc.vector.tensor_tensor(
                                out=hb[:, half * 4:(half + 1) * 4, j * 128:(j + 1) * 128],
                                in0=ps[:, bk, :].rearrange("p (k t) -> p k t", k=4),
                                in1=gfm[:, 3, half * 4:(half + 1) * 4].unsqueeze(2).to_broadcast([128, 4, 128]),
                                op=ALU.mult),
                               reads=[("ps", bk), "gfm"], writes=[("hTe", par, j, half)] + xa)

                pending_sc = []
                pending_g = []

                def expert_pass(e, p, coarse):
                    Wg = od_exp_gate[0, e]
                    Wu = od_exp_up[0, e]
                    Wd = od_exp_down[0, e]
                    guard_i = guard and not coarse
                    par = 0 if coarse else (e % 2)
                    hb = hTe_bufs[par]
                    xa = XH if par == 1 else []
                    if True:
                        jj0 = p * BPP
                        if coarse:
                            for j in range(BPP):
                                gather_block(e, jj0 + j, j, 0, guard_i)
                        seq = []
                        slabs = {}
                        for ui, (c0, ncu) in enumerate(units):
                            for j in range(BPP):
                                seq.append((ui, j))
                        nseq = len(seq)
                        stA = {}

                        def load_unit(ui):
                            c0, ncu = units[ui]
                            if coarse or not cfg.get("wcache", True):
                                slabs[ui] = (col_slab(Wg, c0 * 128, ncu * 128), col_slab(Wu, c0 * 128, ncu * 128),
                                             row_slab(Wd, c0 * 128, ncu))
                            elif st == 0:
                                base = (e * 7 + ui) * 3
                                res = [col_slab(Wg, c0 * 128, ncu * 128), col_slab(Wu, c0 * 128, ncu * 128),
                                       row_slab(Wd, c0 * 128, ncu)]
                                flush_wb()
                                for w_, (v_, key_) in enumerate(res):
                                    wb_pending.append((base + w_, key_))
                                slabs[ui] = tuple(res)
                            else:
                                base = (e * 7 + ui) * 3
                                slabs[ui] = (cache_slab(base, True), cache_slab(base + 1, True),
                                             cache_slab(base + 2, False))

                        def stageA(si):
                            ui, j = seq[si]
                            if j == 0:
                                load_unit(ui)
                            (gsv, gsk), (usv, usk), _ = slabs[ui]
                            jx = si % 2
                            with task(128 * (jj0 + j), guard_i, par):
                                bg = bank("g")
                                bu = bank("u")
                                for (sv, sk, bk_) in ((gsv, gsk, bg), (usv, usk, bu)):
                                    def mm(sv=sv, bk_=bk_, j=j):
                                        ins = None
                                        for k in range(8):
                                            ins = nc.tensor.matmul(ps[:, bk_, :], hb[:, k, j * 128:(j + 1) * 128],
                                                                   sv[:, k, 0:512], start=(k == 0), stop=(k == 7))
                                        return ins
                                    op(PE, mm, reads=[sk, ("hTe", par, j, 0), ("hTe", par, j, 1)] + xa, writes=[("ps", bk_)])
                                op(ACT, lambda bg=bg, jx=jx: nc.scalar.activation(out=sgm[:, jx, :], in_=ps[:, bg, :],
                                                                                  func=AF.Silu),
                                   reads=[("ps", bg)], writes=[("sgm", jx)], force=(not coarse))
                                op(DVE, lambda bu=bu, jx=jx: nc.vector.tensor_tensor(out=a_sl[:, jx, :], in0=ps[:, bu, :],
                                                                                     in1=sgm[:, jx, :], op=ALU.mult),
                                   reads=[("ps", bu), ("sgm", jx)], writes=[("a_sl", jx)])

                        def stageB(si):
                            ui, j = seq[si]
                            jx = si % 2
                            with task(128 * (jj0 + j), guard_i, par):
                                def tr(jx=jx):
                                    ins = None
                                    for c in range(4):
                                        ins = nc.tensor.transpose(psb[:, 0, c * 128:(c + 1) * 128],
                                                                  a_sl[:, jx, c * 128:(c + 1) * 128], identb[:])
                                    return ins
                                op(PE, tr, reads=[("a_sl", jx), "identb"], writes=[("psb", 0)])
                                op(DVE, lambda jx=jx: nc.vector.tensor_copy(out=aTe[:, jx, :], in_=psb[:, 0, 0:512]),
                                   reads=[("psb", 0)], writes=[("aTe", jx)])

                        def stageC(si):
                            ui, j = seq[si]
                            jx = si % 2
                            _, _, (dsv, dsk) = slabs[ui]
                            with task(128 * (jj0 + j), guard_i, par):
                                for dh in range(2):
                                    bd = 4 + dh

                                    def mm(dh=dh, bd=bd, jx=jx, dsv=dsv):
                                        ins = None
                                        for c in range(4):
                                            ins = nc.tensor.matmul(ps[:, bd, :], aTe[:, jx, c * 128:(c + 1) * 128],
                                                                   dsv[:, c, dh * 512:(dh + 1) * 512],
                                                                   start=(c == 0), stop=(c == 3))
                                        return ins
                                    op(PE, mm, reads=[dsk, ("aTe", jx)], writes=[("ps", bd)])
                                    osl = o_sl[:, j, dh * 512:(dh + 1) * 512]
                                    if ui == 0:
                                        op(DVE, lambda osl=osl, bd=bd: nc.vector.tensor_copy(out=osl, in_=ps[:, bd, :]),
                                           reads=[("ps", bd)], writes=[("o_sl", j, dh)])
                                    else:
                                        op(DVE, lambda osl=osl, bd=bd: nc.vector.tensor_tensor(
                                            out=osl, in0=ps[:, bd, :], in1=osl, op=ALU.add),
                                           reads=[("ps", bd), ("o_sl", j, dh)], writes=[("o_sl", j, dh)])

                        for step in range(nseq + 2):
                            if step < nseq:
                                stageA(step)
                            if 0 <= step - 1 < nseq:
                                stageB(step - 1)
                            if 0 <= step - 2 < nseq:
                                stageC(step - 2)
                            if (not coarse) and pending_sc and step >= 2:
                                pending_sc.pop(0)()
                            if (not coarse) and pending_g and step >= 18 and step % 2 == 0:
                                pending_g.pop(0)()
                        if not coarse:
                            while pending_sc:
                                pending_sc.pop(0)()
                            while pending_g:
                                pending_g.pop(0)()
                            for i in range(8):
                                d = i % 2
                                op(DVE, lambda i=i, d=d, e=e: nc.vector.tensor_scalar(
                                    out=Dd[:, d, :], in0=identf[:], scalar1=posm_v[:, i, e:e + 1], scalar2=None,
                                    op0=ALU.mult),
                                   reads=["posm", "identf"], writes=[("Dd", d)])
                                op(PE, lambda i=i, d=d: nc.tensor.matmul(
                                    ps[:, 4 + i // 4, (i % 4) * 128:(i % 4 + 1) * 128], onesf[:], Dd[:, d, :],
                                    start=True, stop=True),
                                   reads=[("Dd", d), "onesf"], writes=[("ps", 4 + i // 4)])
                            for j in range(BPP):
                                op(DVE, lambda j=j: nc.vector.tensor_scalar(
                                    out=STj4[:, j, :], in0=ps[:, 4:6, :].rearrange("p a n -> p (a n)"),
                                    scalar1=iotacol[:, j:j + 1], scalar2=None, op0=ALU.is_equal),
                                   reads=[("ps", 4), ("ps", 5), "iotacol"], writes=[("STj4", j)] + HTK)
                                op(DVE, lambda j=j: nc.vector.tensor_copy(out=o_bf4[:, j, :], in_=o_sl[:, j, :]),
                                   reads=[("o_sl", j, 0), ("o_sl", j, 1)], writes=[("o_bf4", j)] + HTK)
                            for i in range(8):
                                for dh in range(2):
                                    def piece(i=i, dh=dh, e=e):
                                        b = i + 2
                                        bk = bank("s")

                                        def mm(bk=bk, i=i, dh=dh):
                                            ins = None
                                            nj = min(BPP, i + 1)
                                            for j in range(nj):
                                                ins = nc.tensor.matmul(ps[:, bk, :], STj4[:, j, i * 128:(i + 1) * 128],
                                                                       o_bf4[:, j, dh * 512:(dh + 1) * 512],
                                                                       start=(j == 0), stop=(j == nj - 1))
                                            return ins
                                        op(PE, mm, reads=[("STj4", j) for j in range(BPP)] + [("o_bf4", j) for j in range(BPP)] + HTK,
                                           writes=[("ps", bk)])
                                        xs = x_sb[:, b, dh * 512:(dh + 1) * 512]
                                        op(DVE, lambda xs=xs, bk=bk, i=i, e=e: nc.vector.scalar_tensor_tensor(
                                            out=xs, in0=ps[:, bk, :], scalar=comb[:, i, e:e + 1], in1=xs,
                                            op0=ALU.mult, op1=ALU.add),
                                           reads=[("ps", bk), ("x", b, dh), ("comb", i)], writes=[("x", b, dh)])
                                    pending_sc.append(piece)
                            return
                        with task(128 * jj0, guard_i):
                            for i in range(8):
                                d = i % 2
                                op(DVE, lambda i=i, d=d, e=e: nc.vector.tensor_scalar(
                                    out=Dd[:, d, :], in0=identf[:], scalar1=posm_v[:, i, e:e + 1], scalar2=None,
                                    op0=ALU.mult),
                                   reads=["posm", "identf"], writes=[("Dd", d)])
                                op(PE, lambda i=i, d=d: nc.tensor.matmul(
                                    ps[:, 4 + i // 4, (i % 4) * 128:(i % 4 + 1) * 128], onesf[:], Dd[:, d, :],
                                    start=True, stop=True),
                                   reads=[("Dd", d), "onesf"], writes=[("ps", 4 + i // 4)])
                        for j in range(BPP):
                            jj = jj0 + j
                            with task(128 * jj, guard_i):
                                op(DVE, lambda jj=jj: nc.vector.tensor_scalar(
                                    out=STj[:], in0=ps[:, 4:6, :].rearrange("p a n -> p (a n)"),
                                    scalar1=iotacol[:, jj:jj + 1], scalar2=None, op0=ALU.is_equal),
                                   reads=[("ps", 4), ("ps", 5), "iotacol"], writes=["STj"])
                                op(DVE, lambda j=j: nc.vector.tensor_copy(out=o_bf[:], in_=o_sl[:, j, :]),
                                   reads=[("o_sl", j, 0), ("o_sl", j, 1)], writes=["o_bf", ("xn", 1)])
                                for i in range(8):
                                    b = i + 2
                                    for dh in range(2):
                                        bk = bank("s")
                                        op(PE, lambda i=i, dh=dh, bk=bk: nc.tensor.matmul(
                                            ps[:, bk, :], STj[:, i * 128:(i + 1) * 128], o_bf[:, dh * 512:(dh + 1) * 512],
                                            start=True, stop=True),
                                           reads=["STj", "o_bf", ("xn", 1)], writes=[("ps", bk)])
                                        xs = x_sb[:, b, dh * 512:(dh + 1) * 512]
                                        op(DVE, lambda xs=xs, bk=bk, i=i, e=e: nc.vector.scalar_tensor_tensor(
                                            out=xs, in0=ps[:, bk, :], scalar=comb[:, i, e:e + 1], in1=xs,
                                            op0=ALU.mult, op1=ALU.add),
                                           reads=[("ps", bk), ("x", b, dh), ("comb", i)], writes=[("x", b, dh)])

                op(DVE, lambda: nc.vector.memset(o_sl[:].rearrange("p j d -> p (j d)"), 0.0),
                   writes=[("o_sl", j_, dh_) for j_ in range(4) for dh_ in range(2)])
                if guard:
                    for E in (PE, DVE):
                        load_reg(E, n_i[0:1, 0:1], ev_n, 0)
                for j in range(BPP):
                    gather_block(0, j, j, 0, guard)
                for e in range(nexp):
                    if e + 1 < nexp:
                        if guard:
                            for E in (PE, DVE):
                                load_reg(E, n_i[0:1, e + 1:e + 2], ev_n, (e + 1) % 2)
                        for j in range(BPP):
                            pending_g.append(lambda e1=e + 1, j=j: gather_block(e1, j, j, e1 % 2, guard))
                    expert_pass(e, 0, False)
                while pending_sc:
                    pending_sc.pop(0)()
                flush_wb()
                for p in range(1, NPASS):
                    for e in range(nexp):
                        if guard:
                            for E in (PE, ACT, DVE, POOL):
                                load_reg(E, n_i[0:1, e:e + 1], ev_n)
                        with task(128 * p * BPP, guard):
                            expert_pass(e, p, True)
            if stop == "moe":
                dump_x(st)
                continue

            blks = B_29
            for b in blks:
                op(ACT, lambda b=b: nc.scalar.activation(out=junk[:], in_=x_sb[:, b, :], func=AF.Square,
                                                         accum_out=ssq[:, b:b + 1]),
                   reads=xkeys([b]), writes=["STj", ("ssq", b)])
            sk = [("ssq", b) for b in blks]
            rk = [("rstd", b) for b in blks]
            op(DVE, lambda: nc.vector.tensor_scalar(out=rstd[:, 2:NB], in0=ssq[:, 2:NB], scalar1=1.0 / D, scalar2=1e-6,
                                                    op0=ALU.mult, op1=ALU.add), reads=sk, writes=rk)
            op(ACT, lambda: nc.scalar.activation(out=rstd[:, 2:NB], in_=rstd[:, 2:NB], func=AF.Sqrt), reads=rk,
               writes=rk)
            op(DVE, lambda: nc.vector.reciprocal(out=rstd[:, 2:NB], in_=rstd[:, 2:NB]), reads=rk, writes=rk)
            for b in blks:
                j = out_i[0] % 2
                out_i[0] += 1
                op(DVE, lambda b=b, j=j: nc.vector.scalar_tensor_tensor(
                    out=ob[:, j, :], in0=x_sb[:, b, :], scalar=rstd[:, b:b + 1], in1=gfin[:], op0=ALU.mult,
                    op1=ALU.mult),
                   reads=xkeys([b]) + [("rstd", b), "gfin"], writes=[("ob", j)])
                r0 = st * T + (b - 2) * 128
                dma(SP, ost[j], y_d[r0:r0 + 128, :], ob[:, j, :], reads=[("ob", j)])

        for d_ in ost:
            if d_.count:
                SP.wait((d_.sem, d_.count, d_))
        for E in (PE, ACT, DVE):
            if E.count:
                SP.wait((E.sem, E.count, E))
        for d_ in slot_sems + wb_sems + xlds + [xld, cst, cst2]:
            if d_.count:
                SP.wait((d_.sem, d_.count, d_))
    return nc


def _host_consts(core):
    b = core // 4
    s0 = (core % 4) * TOK_CORE
    oh = np.zeros((32, 383), np.float32)
    for i, bkt in enumerate(_BT):
        oh[bkt, i + 64] = 1.0
    kmask = np.zeros((128, 4, NB), np.float32)
    corr = np.ones((128, 4, 4, 16), np.float32)
    for st in range(4):
        start = s0 + st * T
        for blk in range(NB):
            if start - HALO + blk * 128 < 0:
                kmask[:, st, blk] = NEG
        for g, w in enumerate(POOL_W):
            for i in range(16):
                t = start + i
                corr[:, st, g, i] = float(w) / float(min(t + 1, w))
    vmask = np.zeros((128, 4), np.float32)
    vmask[64:, 1] = NEG
    vmask[:64, 2] = NEG
    return {
        "c_oh": oh,
        "c_kmask": kmask.reshape(128, 4 * NB),
        "c_corr": corr.reshape(128, 256),
        "c_ident": np.eye(128, dtype=np.float32),
        "c_vmask": vmask,
        "c_ltri": np.triu(np.ones((128, 128), np.float32), 1),
        "c_iota": np.tile(np.arange(128, dtype=np.float32)[None, :], (128, 1)),
        "c_iotacol": (np.arange(128, dtype=np.float32)[:, None] + 128.0 * np.arange(8, dtype=np.float32)[None, :]),
    }


def kernel(**inputs):
    cfg = CFG
    ncores = cfg["ncores"]
    x = np.asarray(inputs["x"], dtype=np.float32)
    shared = {k: np.ascontiguousarray(np.asarray(v, dtype=np.float32)) for k, v in inputs.items() if k != "x"}
    nc = build_nc(cfg)
    in_maps = []
    for c in range(ncores):
        b = c // 4
        s0 = (c % 4) * TOK_CORE
        xs = np.zeros((TOK_CORE + HALO, D), np.float32)
        if s0 == 0:
            xs[HALO:] = x[b, 0:TOK_CORE]
        else:
            xs[:] = x[b, s0 - HALO:s0 + TOK_CORE]
        m = dict(shared)
        m["x"] = xs
        m.update(_host_consts(c))
        in_maps.append(m)
    res = run_bass_kernel_spmd(nc, in_maps, core_ids=list(range(ncores)))
    out = np.zeros((2, SEQ, D), np.float32)
    for c in range(ncores):
        b = c // 4
        s0 = (c % 4) * TOK_CORE
        out[b, s0:s0 + TOK_CORE] = np.asarray(res.results[c]["y"]).reshape(TOK_CORE, D)
    return out
```

```python
import contextlib
import numpy as np
import concourse.bass as bass
import concourse.mybir as mybir
from concourse.bass_utils import run_bass_kernel_spmd

F32 = mybir.dt.float32
BF16 = mybir.dt.bfloat16
AF = mybir.ActivationFunctionType
ALU = mybir.AluOpType
AX = mybir.AxisListType

D = 1024
T = 1024
HALO = 256
TH = T + HALO
NB = TH // 128
TOK_CORE = 4096
NCORES = 8
SEQ = 16384
DFF0 = 2816
DFFE = 3584
NE = 8
NEG = -1.0e30
NSLOT = 6
POOL_W = (2, 4, 8, 16)

CFG = {"nst": 4, "stop": None, "ncores": 8, "nexp": 8, "sparse": True, "guard": True, "bpp": 4}

_BT_RLE = [[15, 101], [14, 27], [13, 18], [12, 14], [11, 9], [10, 7], [9, 4], [8, 4], [7, 1], [6, 1], [5, 1],
           [4, 1], [3, 1], [2, 1], [1, 1], [0, 1], [17, 1], [18, 1], [19, 1], [20, 1], [21, 1], [22, 1], [23, 1],
           [24, 4], [25, 4], [26, 7], [27, 9], [28, 14], [29, 18]]
_BT = [v for v, n in _BT_RLE for _ in range(n)]
assert len(_BT) == 255


class Eng:
    def __init__(self, name, eng, sem, selfwait=True):
        self.name = name
        self.eng = eng
        self.sem = sem
        self.count = 0
        self.seen = {}
        self.selfwait = selfwait

    def wait(self, ev):
        if ev is None:
            return
        sem, val, owner = ev
        if owner is self:
            if not self.selfwait:
                return
            if val < self.count - 1:
                return
        k = id(sem)
        if self.seen.get(k, 0) >= val:
            return
        self.eng.wait_ge(sem, val)
        self.seen[k] = val


def eng_need(E, ev, seen):
    if ev is None:
        return None
    sem, val, owner = ev
    if owner is E:
        if not E.selfwait:
            return None
        if val < E.count - 1:
            return None
    k = id(sem)
    if seen.get(k, 0) >= val:
        return None
    seen[k] = val
    return (sem, val)


class Tracker:
    def __init__(self):
        self.w = {}
        self.r = {}

    def deps(self, reads, writes):
        evs = []
        for k in reads:
            e = self.w.get(k)
            if e is not None:
                evs.append(e)
        for k in writes:
            e = self.w.get(k)
            if e is not None:
                evs.append(e)
            rr = self.r.get(k)
            if rr:
                evs.extend(rr.values())
        return evs

    def commit(self, ev, who, reads, writes):
        for k in reads:
            self.r.setdefault(k, {})[who] = ev
        for k in writes:
            self.w[k] = ev
            self.r[k] = {}


class DmaSem:
    def __init__(self, name, sem):
        self.name = name
        self.sem = sem
        self.count = 0


def build_nc(cfg):
    nst = cfg["nst"]
    stop = cfg["stop"]
    nexp = cfg["nexp"]
    nc = bass.Bass("TRN2", target_bir_lowering=False)

    def din(name, shape):
        return nc.dram_tensor(name, list(shape), F32, kind="ExternalInput").ap()

    x_d = din("x", [TOK_CORE + HALO, D])
    rel_bias_d = din("rel_bias", [32, 16])
    ev_norm_mix = din("ev_norm_mix", [1, D])
    ev_w_in = din("ev_w_in", [1, D, 2048])
    ev_pool_w = din("ev_pool_w", [1, 4, 128, 128])
    ev_pool_scale = din("ev_pool_scale", [1, 512])
    ev_conv_w = din("ev_conv_w", [1, 3, 512])
    ev_w_out = din("ev_w_out", [1, D, D])
    ev_norm_ffn = din("ev_norm_ffn", [1, D])
    ev_ffn_gate = din("ev_ffn_gate", [1, D, DFF0])
    ev_ffn_up = din("ev_ffn_up", [1, D, DFF0])
    ev_ffn_down = din("ev_ffn_down", [1, DFF0, D])
    od_norm_mix = din("od_norm_mix", [1, D])
    od_w_qkv = din("od_w_qkv", [1, D, 1280])
    od_b_qkv = din("od_b_qkv", [1, 1280])
    od_sinks = din("od_sinks", [1, 16])
    od_w_o = din("od_w_o", [1, D, D])
    od_b_o = din("od_b_o", [1, D])
    od_norm_ffn = din("od_norm_ffn", [1, D])
    od_router = din("od_router", [1, D, NE])
    od_exp_gate = din("od_exp_gate", [1, NE, D, DFFE])
    od_exp_up = din("od_exp_up", [1, NE, D, DFFE])
    od_exp_down = din("od_exp_down", [1, NE, DFFE, D])
    final_norm = din("final_norm", [D])
    c_oh = din("c_oh", [32, 383])
    c_kmask = din("c_kmask", [128, 4 * NB])
    c_corr = din("c_corr", [128, 4 * 4 * 16])
    c_ident = din("c_ident", [128, 128])
    c_vmask = din("c_vmask", [128, 4])
    c_ltri = din("c_ltri", [128, 128])
    c_iota = din("c_iota", [128, 128])
    c_iotacol = din("c_iotacol", [128, 8])
    y_d = nc.dram_tensor("y", [TOK_CORE, D], F32, kind="ExternalOutput").ap()
    wcache = nc.dram_tensor("wcache", [NE * 7 * 3, 128, 4096], BF16, kind="Internal").ap()

    es = contextlib.ExitStack()
    with es:
        def sb(name, shape, dt):
            return es.enter_context(nc.sbuf_tensor(name, list(shape), dt))

        def sem(name):
            return es.enter_context(nc.semaphore(name))

        x_sb = sb("x_sb", [128, NB, D], F32)
        hT = sb("hT", [128, 8, TH], BF16)
        ring = sb("ring", [128, NSLOT, 4096], BF16)
        xn = sb("xn", [128, 2, D], BF16)
        junk = sb("junk", [128, D], BF16)
        ob = sb("ob", [128, 2, D], F32)
        biasT = sb("biasT", [128, 4, 2, 512], F32)
        esink = sb("esink", [128, 2, 512], F32)
        gfin = sb("gfin", [128, D], F32)
        gfm = sb("gfm", [128, 4, 8], F32)
        ssq = sb("ssq", [128, 16], F32)
        rstd = sb("rstd", [128, 16], F32)
        identb = sb("identb", [128, 128], BF16)
        onesb = sb("onesb", [128, 128], BF16)
        bo_row = sb("bo_row", [1, D], BF16)
        bv_row = sb("bv_row", [1, 128], BF16)
        bqk = sb("bqk", [64, 18], F32)
        kmask = sb("kmask", [128, 4 * NB], F32)
        corr = sb("corr", [128, 256], F32)
        vmask = sb("vmask", [128, 4], F32)
        poolw = sb("poolw", [128, 4, 128], BF16)
        pscale = sb("pscale", [128, 4], F32)
        cw = sb("cw", [128, 3, 4], F32)
        wr = sb("wr", [128, 8, NE], BF16)
        oh = sb("oh", [32, 383], F32)
        relb = sb("relb", [32, 16], F32)
        es16 = sb("es16", [128, 16], F32)
        esr = sb("esr", [1, 2, 512], BF16)
        comb = sb("comb", [128, 8, NE], F32)
        rt = sb("rt", [128, 8, 8], F32)
        rs = sb("rs", [128, 16], F32)
        Dd = sb("Dd", [128, 2, 128], F32)
        iota_f = sb("iota_f", [128, 128], F32)
        identf = sb("identf", [128, 128], F32)
        onesf = sb("onesf", [128, 128], F32)
        ltri = sb("ltri", [128, 128], BF16)
        iotacol = sb("iotacol", [128, 8], F32)
        sel_f = sb("sel_f", [128, 64], F32)
        sel_bf = sb("sel_bf", [128, 64], BF16)
        pos_sb = sb("pos_sb", [128, 64], F32)
        posm = sb("posm", [128, 64], F32)
        pshift = sb("pshift", [128, 8], F32)
        n_f = sb("n_f", [1, 8], F32)
        n_i = sb("n_i", [1, 8], mybir.dt.int32)
        dum = sb("dum", [128, 4], F32)
        dumv = sb("dumv", [128, 4], F32)
        dumb = sb("dumb", [2, 2], BF16)
        dumd = sb("dumd", [1, 4], F32)

        UBYTES = 49152
        U = sb("U", [128, UBYTES // 2], BF16)

        def uview(off_b, nbytes, dt):
            a = U[:, off_b // 2:(off_b + nbytes) // 2]
            if dt == F32:
                a = a.bitcast(F32)
            return a

        PADW = 16 + TH
        yT = uview(0, 8 * TH * 2, BF16).rearrange("p (k t) -> p k t", k=8)
        mb = [uview(20480 + i * PADW * 4, PADW * 4, F32) for i in range(3)]
        pooled = uview(20480 + 3 * PADW * 4, TH * 2, BF16)
        TA = TH - 128
        aT = uview(0, 2 * 4 * TA * 2, BF16).rearrange("p (a c t) -> p a c t", a=2, c=4)
        sg = uview(18432, 2 * 512 * 4, F32).rearrange("p (a t) -> p a t", a=2)
        qT = uview(0, 8 * 512 * 2, BF16).rearrange("p (h t) -> p h t", h=8)
        kT = uview(8192, 2 * TH * 2, BF16).rearrange("p (g t) -> p g t", g=2)
        vv = uview(13312, NB * 256 * 2, BF16).rearrange("p (b g e) -> p b g e", b=NB, g=2)
        tbuf = uview(18432, 4 * 512 * 4, F32).rearrange("p (a t) -> p a t", a=4)
        pT = uview(26624, 4 * 512 * 2, BF16).rearrange("p (a t) -> p a t", a=4)
        rden = uview(30720, 512 * 4, F32)
        oT = uview(32768, 8 * T * 2, BF16).rearrange("p (k t) -> p k t", k=8)

        hn = uview(0, 8 * D * 2, BF16).rearrange("p (b d) -> p b d", b=8)
        hTe = uview(16384, 8 * 512 * 2, BF16).rearrange("p (k t) -> p k t", k=8)
        o_sl = uview(24576, 4 * D * 4, F32).rearrange("p (j d) -> p j d", j=4)
        a_sl = uview(40960, 2 * 512 * 2, BF16).rearrange("p (a t) -> p a t", a=2)
        sgm = uview(43008, 2 * 512 * 4, F32).rearrange("p (a t) -> p a t", a=2)
        aTe = uview(47104, 2 * 512 * 2, BF16).rearrange("p (a t) -> p a t", a=2)
        hTe2 = x_sb[:, 0:2, :].rearrange("p b d -> p (b d)").bitcast(BF16).rearrange("p (k t) -> p k t", k=8)
        hTe_bufs = [hTe, hTe2]
        XH = [("x", 0, 0), ("x", 0, 1), ("x", 1, 0), ("x", 1, 1)]
        hT_flat = hT[:].rearrange("p k t -> p (k t)")
        STj4 = hT_flat[:, 0:4096].rearrange("p (j t) -> p j t", j=4)
        o_bf4 = hT_flat[:, 4096:8192].rearrange("p (j t) -> p j t", j=4)
        HTK = [("hT", b_) for b_ in range(NB)]
        Sg = xn[:, 0, :].rearrange("p (i s) -> p i s", i=8)
        o_bf = xn[:, 1, :]
        STj = junk

        ps = es.enter_context(nc.psum_tensor("ps", [128, 6, 512], F32))
        psb = es.enter_context(nc.psum_tensor("psb", [128, 2, 1024], BF16))

        TR = Tracker()
        PE = Eng("pe", nc.tensor, sem("s_pe"), selfwait=False)
        ACT = Eng("act", nc.scalar, sem("s_act"))
        DVE = Eng("dve", nc.vector, sem("s_dve"))
        POOL = Eng("pool", nc.gpsimd, sem("s_pool"))
        SP = Eng("sp", nc.sync, sem("s_sp"))
        slot_sems = [DmaSem("slot%d" % i, sem("s_slot%d" % i)) for i in range(NSLOT)]
        xld = DmaSem("xld", sem("s_xld"))
        xlds = [DmaSem("xld%d" % i, sem("s_xld%d" % i)) for i in range(NB)]
        cst = DmaSem("cst", sem("s_cst"))
        cst2 = DmaSem("cst2", sem("s_cst2"))
        ost = [DmaSem("ost%d" % i, sem("s_ost%d" % i)) for i in range(2)]
        wb_sems = [DmaSem("wb%d" % i, sem("s_wb%d" % i)) for i in range(NSLOT)]
        all_dsems = slot_sems + [xld, cst, cst2] + ost

        REC = {"on": False, "items": {}, "seen": {}}

        def op(E, fn, reads=(), writes=(), force=False):
            if REC["on"] and not force:
                seen = REC["seen"].setdefault(E.name, dict(E.seen))
                waits = []
                for ev in TR.deps(reads, writes):
                    w = eng_need(E, ev, seen)
                    if w is not None:
                        waits.append(w)
                E.count += 1
                ev = (E.sem, E.count, E)
                REC["items"].setdefault(E.name, []).append((waits, fn, None))
                TR.commit(ev, E.name, reads, writes)
                return ev
            for ev in TR.deps(reads, writes):
                E.wait(ev)
            ins = fn()
            E.count += 1
            ins.then_inc(E.sem, 1)
            ev = (E.sem, E.count, E)
            TR.commit(ev, E.name, reads, writes)
            return ev

        def dma(E, ds, out, in_, reads=(), writes=(), **kw):
            if REC["on"]:
                seen = REC["seen"].setdefault(E.name, dict(E.seen))
                waits = []
                for ev in TR.deps(reads, writes):
                    w = eng_need(E, ev, seen)
                    if w is not None:
                        waits.append(w)
                ds.count += 16
                ev = (ds.sem, ds.count, ds)
                REC["items"].setdefault(E.name, []).append(
                    (waits, lambda: E.eng.dma_start(out=out, in_=in_, **kw), (ds, ds.count - 16)))
                TR.commit(ev, "dma:" + ds.name, reads, writes)
                return ev
            for ev in TR.deps(reads, writes):
                E.wait(ev)
            E.eng.dma_start(out=out, in_=in_, **kw).then_inc(ds.sem, 16)
            ds.count += 16
            ev = (ds.sem, ds.count, ds)
            TR.commit(ev, "dma:" + ds.name, reads, writes)
            return ev

        def barrier():
            engs = [PE, ACT, DVE]
            for E in engs:
                for Fo in engs:
                    if Fo is not E and Fo.count > 0:
                        E.wait((Fo.sem, Fo.count, Fo))

        ENG_BY_NAME = {"pe": PE, "act": ACT, "dve": DVE, "pool": POOL, "sp": SP}
        REGS = {}

        def get_reg(E, par=0):
            k_ = (E.name, par)
            if k_ not in REGS:
                REGS[k_] = es.enter_context(E.eng.register("r_%s%d" % (E.name, par)))
            return REGS[k_]

        def emit_dummy(E, n):
            if E is PE:
                ins = nc.tensor.transpose(psb[0:1, 1, 0:2], dumb[0:2, 0:1], identb[0:2, 0:2])
            elif E is ACT:
                ins = nc.scalar.activation(out=dum[:, 0:1], in_=dum[:, 1:2], func=AF.Silu)
            elif E is DVE:
                ins = nc.vector.memset(dumv[:, 2:3], 0.0)
            else:
                raise AssertionError(E.name)
            ins.then_inc(E.sem, n)

        @contextlib.contextmanager
        def task(thr, guard=True, par=0):
            if not guard:
                yield
                return
            assert not REC["on"]
            REC["on"] = True
            REC["items"] = {}
            REC["seen"] = {}
            try:
                yield
            finally:
                REC["on"] = False
            for name, items in REC["items"].items():
                E = ENG_BY_NAME[name]
                reg = get_reg(E, par)
                with E.eng.If_lt(reg, thr + 1):
                    if E is POOL or E is SP:
                        for (_, _, (ds, prev)) in items:
                            if prev > 0:
                                E.eng.wait_ge(ds.sem, prev)
                            E.eng.sem_inc(ds.sem, 16)
                    else:
                        emit_dummy(E, len(items))
                with E.eng.Else():
                    for (waits, fn, ds) in items:
                        for (sm, vl) in waits:
                            E.eng.wait_ge(sm, vl)
                        ins = fn()
                        if ds is None:
                            ins.then_inc(E.sem, 1)
                        else:
                            ins.then_inc(ds[0].sem, 16)

        def load_reg(E, ap, ev, par=0):
            E.wait(ev)
            E.eng.reg_load(get_reg(E, par), ap)

        rot = {"g": [0, 1], "u": [2, 3], "d": [4, 5], "a": [0, 1, 2, 3, 4, 5], "s": [0, 1, 2, 3]}
        rpos = {k: 0 for k in rot}

        def bank(kind):
            b = rot[kind][rpos[kind] % len(rot[kind])]
            rpos[kind] += 1
            return b

        ring_pos = [0]

        def col_slab(Wap, c0, ncols):
            s = ring_pos[0] % NSLOT
            ring_pos[0] += 1
            view = ring[:, s, 0:8 * ncols].rearrange("p (k n) -> p k n", k=8)
            dma(POOL, slot_sems[s], view, Wap[:, c0:c0 + ncols].rearrange("(k p) n -> p k n", p=128),
                writes=[("slab", s)])
            return view, ("slab", s)

        def row_slab(Wap, r0, nch):
            s = ring_pos[0] % NSLOT
            ring_pos[0] += 1
            view = ring[:, s, 0:nch * 1024].rearrange("p (c n) -> p c n", c=nch)
            dma(POOL, slot_sems[s], view, Wap[r0:r0 + nch * 128, :].rearrange("(c p) n -> p c n", p=128),
                writes=[("slab", s)])
            return view, ("slab", s)

        wb_pending = []

        def flush_wb():
            while wb_pending:
                idx_, key_ = wb_pending.pop(0)
                s_ = key_[1]
                dma(POOL, wb_sems[s_], wcache[idx_], ring[:, s_, :], reads=[key_], writes=[("wc", idx_)])

        def cache_slab(idx, is_col):
            s_ = ring_pos[0] % NSLOT
            ring_pos[0] += 1
            if is_col:
                view = ring[:, s_, :].rearrange("p (k n) -> p k n", k=8)
            else:
                view = ring[:, s_, :].rearrange("p (c n) -> p c n", c=4)
            dma(POOL, slot_sems[s_], ring[:, s_, :], wcache[idx], reads=[("wc", idx)], writes=[("slab", s_)])
            return view, ("slab", s_)

        def blocks_of(t0, tn):
            return list(range(t0 // 128, (t0 + tn + 127) // 128))

        def hkeys(t0, tn):
            return [("hT", b) for b in blocks_of(t0, tn)]

        def cload(out, in_, key, eng=None, ds=None, **kw):
            dma(eng or SP, ds or cst, out, in_, writes=[key], **kw)

        norm_vecs = [ev_norm_mix[0], ev_norm_ffn[0], od_norm_mix[0], od_norm_ffn[0]]
        for i, v in enumerate(norm_vecs):
            cload(gfm[:, i, :], v.rearrange("(k p) -> p k", p=128), "gfm", allow_slow_non_contiguous=True)
        cload(gfin[:], final_norm.partition_broadcast(128), "gfin")
        cload(identb[:], c_ident, "identb", eng=POOL, ds=cst2)
        cload(bo_row[:], od_b_o, "bo_row", eng=POOL, ds=cst2)
        cload(bv_row[:], od_b_qkv[:, 1152:1280], "bv_row", eng=POOL, ds=cst2)
        cload(bqk[:], od_b_qkv[0, 0:1152].rearrange("(h d) -> d h", d=64), "bqk", allow_slow_non_contiguous=True)
        cload(kmask[:], c_kmask, "kmask")
        cload(corr[:], c_corr, "corr")
        cload(vmask[:], c_vmask, "vmask")
        cload(poolw[:], ev_pool_w[0].rearrange("g c d -> c g d"), "poolw", eng=POOL, ds=cst2)
        cload(pscale[:], ev_pool_scale[0].rearrange("(g p) -> p g", p=128), "pscale", allow_slow_non_contiguous=True)
        cload(cw[:], ev_conv_w[0].rearrange("k (j p) -> p k j", p=128), "cw", allow_slow_non_contiguous=True)
        cload(wr[:], od_router[0].rearrange("(k p) e -> p k e", p=128), "wr", eng=POOL, ds=cst2)
        cload(oh[:], c_oh, "oh")
        cload(relb[:], rel_bias_d, "relb")
        cload(es16[:], od_sinks[0].partition_broadcast(128), "es16")
        cload(iota_f[:], c_iota, "iota_f")
        cload(identf[:], c_ident, "identf")
        cload(iotacol[:], c_iotacol, "iotacol")
        cload(ltri[:], c_ltri, "ltri", eng=POOL, ds=cst2)

        for k_ in list(TR.w.keys()):
            ev_ = TR.w[k_]
            if ev_[2] is cst:
                TR.w[k_] = (cst.sem, cst.count, cst)
            elif ev_[2] is cst2:
                TR.w[k_] = (cst2.sem, cst2.count, cst2)
        op(DVE, lambda: nc.vector.memset(onesb[:], 1.0), writes=["onesb"])
        op(DVE, lambda: nc.vector.memset(onesf[:], 1.0), writes=["onesf"])
        ev_da = op(DVE, lambda: nc.vector.memset(dum[:], 0.0), writes=["dum"])
        op(DVE, lambda: nc.vector.memset(dumv[:], 0.0), writes=["dumv"])
        ev_db = op(DVE, lambda: nc.vector.memset(dumb[:], 0.0), writes=["dumb"])
        ACT.wait(ev_da)
        PE.wait(ev_db)
        for i in range(3):
            op(DVE, lambda i=i: nc.vector.memset(mb[i][:, 0:16], 0.0), writes=[("mbpad", i)])

        op(ACT, lambda: nc.scalar.activation(out=es16[:], in_=es16[:], func=AF.Exp), reads=["es16"], writes=["es16"])
        for g in range(2):
            op(DVE, lambda g=g: nc.vector.tensor_copy(
                out=esink[:, g, :].rearrange("p (h q) -> p h q", h=8),
                in_=es16[:, g * 8:(g + 1) * 8].unsqueeze(2).to_broadcast([128, 8, 64])),
               reads=["es16"], writes=["esink"])

        for g in range(2):
            op(DVE, lambda g=g: nc.vector.tensor_copy(
                out=esr[0:1, g, :].rearrange("p (h q) -> p h q", h=8),
                in_=es16[0:1, g * 8:(g + 1) * 8].unsqueeze(2).to_broadcast([1, 8, 64])),
               reads=["es16"], writes=["esr"])
        offs = [-128, 0, -192, -64]
        for v in range(4):
            b0 = bank("a")
            b1 = bank("a")
            assert b1 == b0 + 1

            def mm_bias(v=v, b0=b0):
                ins = None
                for q in range(64):
                    s0 = 255 + offs[v] - q
                    dst = ps[:, b0 + q // 32, (q % 32) * 16:(q % 32) * 16 + 16]
                    ins = nc.tensor.matmul(dst, oh[:, s0:s0 + 128], relb[:], start=True, stop=True)
                return ins
            op(PE, mm_bias, reads=["oh", "relb"], writes=[("ps", b0), ("ps", b1)])
            for g in range(2):
                def cp(v=v, g=g, b0=b0):
                    src = ps[:, b0:b0 + 2, :].rearrange("p a (q h) -> p (a q) h", h=16)[:, :, g * 8:(g + 1) * 8]
                    return nc.vector.tensor_scalar(
                        out=biasT[:, v, g, :].rearrange("p (h q) -> p h q", h=8),
                        in0=src.rearrange("p q h -> p h q"),
                        scalar1=vmask[:, v:v + 1], scalar2=None, op0=ALU.add)
                op(DVE, cp, reads=[("ps", b0), ("ps", b0 + 1), "vmask"], writes=["biasT"])

        def rmsnorm_to_hT(ni, blks, keep=False):
            for b in blks:
                op(ACT, lambda b=b: nc.scalar.activation(out=junk[:], in_=x_sb[:, b, :], func=AF.Square,
                                                         accum_out=ssq[:, b:b + 1]),
                   reads=[("x", b, 0), ("x", b, 1)], writes=["STj", ("ssq", b)])
            b0, b1 = blks[0], blks[-1] + 1
            sk = [("ssq", b) for b in blks]
            rk = [("rstd", b) for b in blks]
            op(DVE, lambda: nc.vector.tensor_scalar(out=rstd[:, b0:b1], in0=ssq[:, b0:b1], scalar1=1.0 / D,
                                                    scalar2=1e-6, op0=ALU.mult, op1=ALU.add),
               reads=sk, writes=rk)
            op(ACT, lambda: nc.scalar.activation(out=rstd[:, b0:b1], in_=rstd[:, b0:b1], func=AF.Sqrt),
               reads=rk, writes=rk)
            op(DVE, lambda: nc.vector.reciprocal(out=rstd[:, b0:b1], in_=rstd[:, b0:b1]), reads=rk, writes=rk)
            for i, b in enumerate(blks):
                j = i % 2
                if keep:
                    xsrc = hn[:, b - 2, :]
                    xkey = ("hn", b - 2)
                else:
                    xsrc = xn[:, j, :]
                    xkey = ("xn", j)
                if i % 2 == 0:
                    op(ACT, lambda b=b, xsrc=xsrc: nc.scalar.activation(out=xsrc, in_=x_sb[:, b, :], func=AF.Copy,
                                                                        scale=rstd[:, b:b + 1]),
                       reads=[("x", b, 0), ("x", b, 1), ("rstd", b)], writes=[xkey])
                else:
                    op(DVE, lambda b=b, xsrc=xsrc: nc.vector.tensor_scalar(out=xsrc, in0=x_sb[:, b, :],
                                                                           scalar1=rstd[:, b:b + 1], scalar2=None,
                                                                           op0=ALU.mult),
                       reads=[("x", b, 0), ("x", b, 1), ("rstd", b)], writes=[xkey])

                def tr(xsrc=xsrc):
                    ins = None
                    for k in range(8):
                        ins = nc.tensor.transpose(psb[:, 0, k * 128:(k + 1) * 128], xsrc[:, k * 128:(k + 1) * 128],
                                                  identb[:])
                    return ins
                op(PE, tr, reads=[xkey, "identb"], writes=[("psb", 0)])
                op(DVE, lambda b=b: nc.vector.tensor_tensor(
                    out=hT[:, :, b * 128:(b + 1) * 128],
                    in0=psb[:, 0, :].rearrange("p (k t) -> p k t", k=8),
                    in1=gfm[:, ni, :].unsqueeze(2).to_broadcast([128, 8, 128]), op=ALU.mult),
                   reads=[("psb", 0), "gfm"], writes=[("hT", b)])

        def proj_fm(slab, skey, col0, t0, tn, M=128, bk=None):
            b = bank("a") if bk is None else bk

            def mm():
                ins = None
                for k in range(8):
                    ins = nc.tensor.matmul(ps[0:M, b, 0:tn], slab[:, k, col0:col0 + M], hT[:, k, t0:t0 + tn],
                                           start=(k == 0), stop=(k == 7))
                return ins
            op(PE, mm, reads=[skey] + hkeys(t0, tn), writes=[("ps", b)])
            return b

        def down_proj(A_fn, akeys_fn, slabs, nch, blks, tok_off, comb_fn=None, bias_row=None):
            for b in blks:
                t0 = b * 128 - tok_off
                for dh in range(2):
                    bk = bank("d")
                    rk = list(akeys_fn(b))
                    for (_, sk, _) in slabs:
                        rk.append(sk)
                    if bias_row is not None:
                        rk.append("bo_row")
                        rk.append("onesb")

                    def mm(b=b, dh=dh, bk=bk, t0=t0):
                        ins = None
                        first = True
                        if bias_row is not None:
                            ins = nc.tensor.matmul(ps[:, bk, :], onesb[0:1, :], bias_row[0:1, dh * 512:(dh + 1) * 512],
                                                   start=True, stop=False)
                            first = False
                        c = 0
                        for (sv, _, ns) in slabs:
                            for cc in range(ns):
                                ins = nc.tensor.matmul(ps[:, bk, :], A_fn(c, t0), sv[:, cc, dh * 512:(dh + 1) * 512],
                                                       start=first, stop=(c == nch - 1))
                                first = False
                                c += 1
                        return ins
                    op(PE, mm, reads=rk, writes=[("ps", bk)])
                    xs = x_sb[:, b, dh * 512:(dh + 1) * 512]
                    if comb_fn is None:
                        op(DVE, lambda bk=bk, xs=xs: nc.vector.tensor_tensor(out=xs, in0=ps[:, bk, :], in1=xs,
                                                                             op=ALU.add),
                           reads=[("ps", bk), ("x", b, dh)], writes=[("x", b, dh)])
                    else:
                        cap, ckey = comb_fn(b)
                        op(DVE, lambda bk=bk, xs=xs, cap=cap: nc.vector.scalar_tensor_tensor(
                            out=xs, in0=ps[:, bk, :], scalar=cap, in1=xs, op0=ALU.mult, op1=ALU.add),
                           reads=[("ps", bk), ("x", b, dh), ckey], writes=[("x", b, dh)])

        def ffn(Wg, Wu, Wd, nch, tiles, blks, comb_fn):
            units = [(c0, min(4, nch - c0)) for c0 in range(0, nch, 4)]
            pend = None
            sgi = [0]

            def emit_down(pd):
                dsv, dsk, ab, ncu = pd

                def A_fn(c, t0, ab=ab):
                    return aT[:, ab, c, t0 - 128:t0 - 128 + 128]

                def akeys(b, ab=ab, ncu=ncu):
                    ti = None
                    for i, (t0, tn) in enumerate(tiles):
                        if t0 <= b * 128 < t0 + tn:
                            ti = i
                    return [("aT", ab, c, ti) for c in range(ncu)]
                down_proj(A_fn, akeys, [(dsv, dsk, ncu)], ncu, blks, 0, comb_fn=comb_fn)

            for ui, (c0, ncu) in enumerate(units):
                gsv, gsk = col_slab(Wg, c0 * 128, ncu * 128)
                usv, usk = col_slab(Wu, c0 * 128, ncu * 128)
                dsv, dsk = row_slab(Wd, c0 * 128, ncu)
                ab = ui % 2
                for cc in range(ncu):
                    for ti, (t0, tn) in enumerate(tiles):
                        bg = bank("g")
                        bu = bank("u")
                        proj_fm(gsv, gsk, cc * 128, t0, tn, bk=bg)
                        proj_fm(usv, usk, cc * 128, t0, tn, bk=bu)
                        j = sgi[0] % 2
                        sgi[0] += 1
                        op(ACT, lambda bg=bg, j=j, tn=tn: nc.scalar.activation(out=sg[:, j, 0:tn], in_=ps[:, bg, 0:tn],
                                                                               func=AF.Silu),
                           reads=[("ps", bg)], writes=[("sg", j)])
                        op(DVE, lambda bu=bu, j=j, tn=tn, t0=t0, ab=ab, cc=cc: nc.vector.tensor_tensor(
                            out=aT[:, ab, cc, t0 - 128:t0 - 128 + tn], in0=ps[:, bu, 0:tn], in1=sg[:, j, 0:tn],
                            op=ALU.mult),
                           reads=[("ps", bu), ("sg", j)], writes=[("aT", ab, cc, ti)])
                if pend is not None:
                    emit_down(pend)
                pend = (dsv, dsk, ab, ncu)
            emit_down(pend)

        def xkeys(blks):
            return [("x", b, dh) for b in blks for dh in range(2)]

        TT_ALL = [(0, 512), (512, 512), (1024, 256)]
        TT_19 = [(128, 384), (512, 384), (896, 384)]
        TT_29 = [(256, 512), (768, 512)]
        B_ALL = list(range(NB))
        B_19 = list(range(1, NB))
        B_29 = list(range(2, NB))
        out_i = [0]

        def dump_x(st):
            for b in B_29:
                j = out_i[0] % 2
                out_i[0] += 1
                op(DVE, lambda b=b, j=j: nc.vector.tensor_copy(out=ob[:, j, :], in_=x_sb[:, b, :]),
                   reads=xkeys([b]), writes=[("ob", j)])
                r0 = st * T + (b - 2) * 128
                dma(SP, ost[j], y_d[r0:r0 + 128, :], ob[:, j, :], reads=[("ob", j)])

        for st in range(nst):
            for b_ in B_ALL:
                dma(SP, xlds[b_], x_sb[:, b_, :], x_d[st * T + b_ * 128:st * T + (b_ + 1) * 128, :],
                    writes=xkeys([b_]))

            rmsnorm_to_hT(0, B_ALL)
            barrier()
            wslabs = [col_slab(ev_w_in[0], i * 512, 512) for i in range(4)]
            for g in range(4):
                for (t0, tn) in TT_ALL:
                    bk = proj_fm(wslabs[0][0], wslabs[0][1], g * 128, t0, tn)
                    op(ACT, lambda bk=bk, t0=t0, tn=tn: nc.scalar.copy(out=mb[0][:, 16 + t0:16 + t0 + tn],
                                                                       in_=ps[:, bk, 0:tn]),
                       reads=[("ps", bk)], writes=[("mb", 0)])
                src = 0
                dst = 1
                sh = 1
                for lvl in range(g + 1):
                    op(DVE, lambda src=src, dst=dst, sh=sh: nc.vector.tensor_tensor(
                        out=mb[dst][:, 16:16 + TH], in0=mb[src][:, 16:16 + TH], in1=mb[src][:, 16 - sh:16 - sh + TH],
                        op=ALU.add),
                       reads=[("mb", src), ("mbpad", src)], writes=[("mb", dst)])
                    src = dst
                    dst = 2 if dst == 1 else 1
                    sh *= 2
                op(DVE, lambda src=src, g=g, st=st: nc.vector.tensor_tensor(
                    out=mb[src][:, 16 + HALO:16 + HALO + 16], in0=mb[src][:, 16 + HALO:16 + HALO + 16],
                    in1=corr[:, (st * 4 + g) * 16:(st * 4 + g) * 16 + 16], op=ALU.mult),
                   reads=[("mb", src), "corr"], writes=[("mb", src)])
                op(DVE, lambda src=src, g=g: nc.vector.scalar_tensor_tensor(
                    out=pooled[:, :], in0=mb[src][:, 16:16 + TH], scalar=1.0 / POOL_W[g], in1=mb[0][:, 16:16 + TH],
                    op0=ALU.mult, op1=ALU.subtract),
                   reads=[("mb", src), ("mb", 0)], writes=["pooled"])
                for (t0, tn) in TT_ALL:
                    bk = bank("a")
                    op(PE, lambda bk=bk, t0=t0, tn=tn, g=g: nc.tensor.matmul(
                        ps[:, bk, 0:tn], poolw[:, g, :], pooled[:, t0:t0 + tn], start=True, stop=True),
                       reads=["poolw", "pooled"], writes=[("ps", bk)])
                    op(ACT, lambda bk=bk, t0=t0, tn=tn, g=g: nc.scalar.activation(
                        out=yT[:, g, t0:t0 + tn], in_=ps[:, bk, 0:tn], func=AF.Copy, scale=pscale[:, g:g + 1]),
                       reads=[("ps", bk), "pscale"], writes=[("yT", g)])
            for j in range(4):
                for (t0, tn) in TT_ALL:
                    bk = proj_fm(wslabs[2][0], wslabs[2][1], j * 128, t0, tn)
                    op(ACT, lambda bk=bk, t0=t0, tn=tn: nc.scalar.copy(out=mb[0][:, 16 + t0:16 + t0 + tn],
                                                                       in_=ps[:, bk, 0:tn]),
                       reads=[("ps", bk)], writes=[("mb", 0)])
                for (t0, tn) in TT_ALL:
                    bk = proj_fm(wslabs[3][0], wslabs[3][1], j * 128, t0, tn)
                    op(DVE, lambda bk=bk, t0=t0, tn=tn: nc.vector.tensor_tensor(
                        out=mb[1][:, 16 + t0:16 + t0 + tn], in0=ps[:, bk, 0:tn], in1=mb[0][:, 16 + t0:16 + t0 + tn],
                        op=ALU.mult),
                       reads=[("ps", bk), ("mb", 0)], writes=[("mb", 1)])
                op(DVE, lambda j=j: nc.vector.tensor_scalar(out=mb[2][:, 16:16 + TH], in0=mb[1][:, 16:16 + TH],
                                                            scalar1=cw[:, 2, j:j + 1], scalar2=None, op0=ALU.mult),
                   reads=[("mb", 1), "cw"], writes=[("mb", 2)])
                for kk in (1, 0):
                    shf = 2 - kk
                    op(DVE, lambda j=j, kk=kk, shf=shf: nc.vector.scalar_tensor_tensor(
                        out=mb[2][:, 16:16 + TH], in0=mb[1][:, 16 - shf:16 - shf + TH], scalar=cw[:, kk, j:j + 1],
                        in1=mb[2][:, 16:16 + TH], op0=ALU.mult, op1=ALU.add),
                       reads=[("mb", 1), ("mbpad", 1), ("mb", 2), "cw"], writes=[("mb", 2)])
                for (t0, tn) in TT_ALL:
                    bk = proj_fm(wslabs[1][0], wslabs[1][1], j * 128, t0, tn)
                    op(DVE, lambda bk=bk, t0=t0, tn=tn, j=j: nc.vector.tensor_tensor(
                        out=yT[:, 4 + j, t0:t0 + tn], in0=ps[:, bk, 0:tn], in1=mb[2][:, 16 + t0:16 + t0 + tn],
                        op=ALU.mult),
                       reads=[("ps", bk), ("mb", 2)], writes=[("yT", 4 + j)])
            wo = [row_slab(ev_w_out[0], i * 512, 4) for i in range(2)]
            down_proj(lambda c, t0: yT[:, c, t0:t0 + 128], lambda b: [("yT", c) for c in range(8)],
                      [(wo[0][0], wo[0][1], 4), (wo[1][0], wo[1][1], 4)], 8, B_19, 0)
            if stop == "mixer":
                dump_x(st)
                continue

            rmsnorm_to_hT(1, B_19)
            barrier()
            ffn(ev_ffn_gate[0], ev_ffn_up[0], ev_ffn_down[0], DFF0 // 128, TT_19, B_19, None)
            if stop == "ffn0":
                dump_x(st)
                continue

            rmsnorm_to_hT(2, B_19)
            barrier()
            wq = [col_slab(od_w_qkv[0], 0, 512), col_slab(od_w_qkv[0], 512, 512), col_slab(od_w_qkv[0], 1024, 256)]
            for g in range(2):
                for (t0, tn) in TT_19:
                    bk = proj_fm(wq[2][0], wq[2][1], g * 64, t0, tn, M=64)
                    op(ACT, lambda bk=bk, g=g, t0=t0, tn=tn: nc.scalar.activation(
                        out=kT[0:64, g, t0:t0 + tn], in_=ps[0:64, bk, 0:tn], func=AF.Identity,
                        bias=bqk[:, 16 + g:17 + g]),
                       reads=[("ps", bk), "bqk"], writes=[("kT", g)])
            for b in B_19:
                bk = bank("a")

                def mmv(b=b, bk=bk):
                    nc.tensor.matmul(ps[:, bk, 0:128], onesb[0:1, :], bv_row[0:1, :], start=True, stop=False)
                    ins = None
                    for k in range(8):
                        ins = nc.tensor.matmul(ps[:, bk, 0:128], hT[:, k, b * 128:(b + 1) * 128],
                                               wq[2][0][:, k, 128:256], start=False, stop=(k == 7))
                    return ins
                op(PE, mmv, reads=[wq[2][1], ("hT", b), "onesb", "bv_row"], writes=[("ps", bk)])
                op(DVE, lambda b=b, bk=bk: nc.vector.tensor_copy(
                    out=vv[:, b, :, :].rearrange("p g (e d) -> p g e d", e=2),
                    in_=ps[:, bk, 0:128].rearrange("p (g d) -> p g d", g=2).unsqueeze(2).to_broadcast([128, 2, 2, 64])),
                   reads=[("ps", bk)], writes=[("vv", b)])
            for sub in range(2):
                tq0 = HALO + sub * 512
                for g in range(2):
                    for h8 in range(8):
                        h = g * 8 + h8
                        bk = proj_fm(wq[h // 8][0], wq[h // 8][1], (h % 8) * 64, tq0, 512, M=64)
                        op(ACT, lambda bk=bk, h=h, h8=h8: nc.scalar.activation(
                            out=qT[0:64, h8, :], in_=ps[0:64, bk, :], func=AF.Identity, bias=bqk[:, h:h + 1]),
                           reads=[("ps", bk), "bqk"], writes=[("qT", h8)])
                    def att_s1(ci, g=g, tq0=tq0, sub=sub):
                        q0 = tq0 + ci * 64
                        odd = (q0 // 64) % 2
                        blkA = (q0 - 128) // 128
                        blkB = blkA + 1
                        var = (2, 3) if odd else (0, 1)
                        sb_ = []
                        for ii, blk in enumerate((blkA, blkB)):
                            bk = bank("a")
                            sb_.append(bk)
                            op(PE, lambda bk=bk, blk=blk, g=g, ci=ci: nc.tensor.matmul(
                                ps[:, bk, :], kT[0:64, g, blk * 128:(blk + 1) * 128],
                                qT[0:64, :, ci * 64:(ci + 1) * 64], start=True, stop=True),
                               reads=[("kT", g)] + [("qT", i) for i in range(8)], writes=[("ps", bk)])
                        tj = [(2 * ci + ii) % 4 for ii in range(2)]
                        for ii, blk in enumerate((blkA, blkB)):
                            op(DVE, lambda ii=ii, bk=sb_[ii], v=var[ii], g=g, j=tj[ii]: nc.vector.scalar_tensor_tensor(
                                out=tbuf[:, j, :], in0=ps[:, bk, :], scalar=0.125, in1=biasT[:, v, g, :],
                                op0=ALU.mult, op1=ALU.add),
                               reads=[("ps", sb_[ii]), "biasT"], writes=[("tbuf", tj[ii])])
                            op(ACT, lambda ii=ii, blk=blk, j=tj[ii], st=st: nc.scalar.activation(
                                out=pT[:, j, :], in_=tbuf[:, j, :], func=AF.Exp,
                                bias=kmask[:, st * NB + blk:st * NB + blk + 1]),
                               reads=[("tbuf", tj[ii]), "kmask"], writes=[("pT", tj[ii])])
                        return (ci, blkA, blkB, tj)

                    def att_s2(state, g=g, sub=sub):
                        ci, blkA, blkB, tj = state
                        bo = bank("a")
                        bd = bank("a")

                        def mmo(bo=bo, blkA=blkA, blkB=blkB, g=g, tj=tj):
                            nc.tensor.matmul(ps[:, bo, :], vv[:, blkA, g, :], pT[:, tj[0], :], start=True, stop=False)
                            return nc.tensor.matmul(ps[:, bo, :], vv[:, blkB, g, :], pT[:, tj[1], :], start=False,
                                                    stop=True)
                        op(PE, mmo, reads=[("vv", blkA), ("vv", blkB), ("pT", tj[0]), ("pT", tj[1])],
                           writes=[("ps", bo)])

                        def mmd(bd=bd, tj=tj, g=g):
                            nc.tensor.matmul(ps[:, bd, :], onesb[0:1, :], esr[0:1, g, :], start=True, stop=False)
                            nc.tensor.matmul(ps[:, bd, :], onesb[:, :], pT[:, tj[0], :], start=False, stop=False)
                            return nc.tensor.matmul(ps[:, bd, :], onesb[:, :], pT[:, tj[1], :], start=False, stop=True)
                        op(PE, mmd, reads=["onesb", "esr", ("pT", tj[0]), ("pT", tj[1])], writes=[("ps", bd)])
                        op(ACT, lambda bd=bd: nc.scalar.activation(out=rden[:, :], in_=ps[:, bd, :], func=AF.Ln),
                           reads=[("ps", bd)], writes=["rden"])
                        op(ACT, lambda: nc.scalar.activation(out=rden[:, :], in_=rden[:, :], func=AF.Exp, scale=-1.0),
                           reads=["rden"], writes=["rden"])
                        tok0 = sub * 512 + ci * 64
                        for half in range(2):
                            pr = slice(half * 64, half * 64 + 64)
                            op(DVE, lambda half=half, pr=pr, bo=bo, g=g, tok0=tok0: nc.vector.tensor_tensor(
                                out=oT[pr, g * 4:(g + 1) * 4, tok0:tok0 + 64],
                                in0=ps[pr, bo, :].rearrange("p (a e q) -> p a e q", a=4, e=2)[:, :, half, :],
                                in1=rden[pr, :].rearrange("p (a e q) -> p a e q", a=4, e=2)[:, :, half, :],
                                op=ALU.mult),
                               reads=[("ps", bo), "rden"], writes=[("oT", g, tok0 // 128)])

                    states = {}
                    for step in range(9):
                        if step < 8:
                            states[step] = att_s1(step)
                        if step >= 1:
                            att_s2(states[step - 1])

            wos = [row_slab(od_w_o[0], i * 512, 4) for i in range(2)]
            down_proj(lambda c, t0: oT[:, c, t0:t0 + 128],
                      lambda b: [("oT", g, b - 2) for g in range(2)],
                      [(wos[0][0], wos[0][1], 4), (wos[1][0], wos[1][1], 4)], 8, B_29, HALO, bias_row=bo_row)
            if stop == "attn":
                dump_x(st)
                continue

            sparse = cfg.get("sparse", True)
            rmsnorm_to_hT(3, B_29, keep=sparse)
            barrier()
            for b in B_29:
                i = b - 2
                bk = bank("a")

                def mmr(b=b, bk=bk):
                    ins = None
                    for k in range(8):
                        ins = nc.tensor.matmul(ps[:, bk, 0:NE], hT[:, k, b * 128:(b + 1) * 128], wr[:, k, :],
                                               start=(k == 0), stop=(k == 7))
                    return ins
                op(PE, mmr, reads=[("hT", b), "wr"], writes=[("ps", bk)])
                lg = rt[:, 0, :]
                e1 = rt[:, 1, :]
                l2 = rt[:, 2, :]
                sel = rt[:, 3, :]
                ex = rt[:, 4, :]
                m1 = rs[:, 0:1]
                m2 = rs[:, 1:2]
                nm1 = rs[:, 2:3]
                den = rs[:, 3:4]
                K = "rt"
                op(DVE, lambda bk=bk: nc.vector.tensor_copy(out=lg, in_=ps[:, bk, 0:NE]), reads=[("ps", bk)], writes=[K])
                op(DVE, lambda: nc.vector.reduce_max(out=m1, in_=lg, axis=AX.X), reads=[K], writes=[K])
                op(DVE, lambda: nc.vector.tensor_scalar(out=e1, in0=lg, scalar1=m1, scalar2=NEG, op0=ALU.is_equal,
                                                        op1=ALU.mult), reads=[K], writes=[K])
                op(DVE, lambda: nc.vector.tensor_tensor(out=l2, in0=e1, in1=lg, op=ALU.add), reads=[K], writes=[K])
                op(DVE, lambda: nc.vector.reduce_max(out=m2, in_=l2, axis=AX.X), reads=[K], writes=[K])
                op(DVE, lambda: nc.vector.tensor_scalar(out=sel, in0=lg, scalar1=m2, scalar2=None, op0=ALU.is_ge),
                   reads=[K], writes=[K])
                op(DVE, lambda: nc.vector.tensor_scalar(out=nm1, in0=m1, scalar1=-1.0, scalar2=None, op0=ALU.mult),
                   reads=[K], writes=[K])
                op(ACT, lambda: nc.scalar.activation(out=ex, in_=lg, func=AF.Exp, bias=nm1), reads=[K], writes=[K])
                op(DVE, lambda: nc.vector.tensor_tensor(out=ex, in0=ex, in1=sel, op=ALU.mult), reads=[K], writes=[K])
                op(DVE, lambda: nc.vector.reduce_sum(out=den, in_=ex, axis=AX.X), reads=[K], writes=[K])
                op(DVE, lambda: nc.vector.reciprocal(out=den, in_=den), reads=[K], writes=[K])
                op(DVE, lambda i=i: nc.vector.tensor_scalar(out=comb[:, i, :], in0=ex, scalar1=den, scalar2=None,
                                                            op0=ALU.mult), reads=[K], writes=[("comb", i)])
                if sparse:
                    op(DVE, lambda i=i: nc.vector.tensor_copy(out=sel_f[:, i * 8:(i + 1) * 8], in_=sel), reads=[K],
                       writes=[("sel_f", i)])
                    op(DVE, lambda i=i: nc.vector.tensor_copy(out=sel_bf[:, i * 8:(i + 1) * 8], in_=sel), reads=[K],
                       writes=[("sel_bf", i)])
            if not sparse:
                for e in range(nexp):
                    ffn(od_exp_gate[0, e], od_exp_up[0, e], od_exp_down[0, e], DFFE // 128, TT_29, B_29,
                        lambda b, e=e: (comb[:, b - 2, e:e + 1], ("comb", b - 2)))
            else:
                guard = cfg.get("guard", True)
                BPP = cfg.get("bpp", 4)
                NPASS = 8 // BPP
                bkp = bank("a")
                bkn = bank("a")
                selk = [("sel_bf", i) for i in range(8)]

                def mmpos(bkp=bkp):
                    ins = None
                    for i in range(8):
                        ins = nc.tensor.matmul(ps[:, bkp, i * 8:(i + 1) * 8], ltri[:], sel_bf[:, i * 8:(i + 1) * 8],
                                               start=True, stop=(i == 0))
                        for i2 in range(i):
                            ins = nc.tensor.matmul(ps[:, bkp, i * 8:(i + 1) * 8], onesb[:], sel_bf[:, i2 * 8:(i2 + 1) * 8],
                                                   start=False, stop=(i2 == i - 1))
                    return ins
                op(PE, mmpos, reads=selk + ["ltri", "onesb"], writes=[("ps", bkp)])

                def mmn(bkn=bkn):
                    ins = None
                    for i in range(8):
                        ins = nc.tensor.matmul(ps[0:1, bkn, 0:8], onesb[:, 0:1], sel_bf[:, i * 8:(i + 1) * 8],
                                               start=(i == 0), stop=(i == 7))
                    return ins
                op(PE, mmn, reads=selk + ["onesb"], writes=[("ps", bkn)])
                op(DVE, lambda: nc.vector.tensor_scalar(out=pos_sb[:], in0=ps[:, bkp, 0:64], scalar1=1.0, scalar2=None,
                                                        op0=ALU.add), reads=[("ps", bkp)], writes=["pos_sb"])
                op(DVE, lambda: nc.vector.tensor_tensor(out=pos_sb[:], in0=pos_sb[:], in1=sel_f[:], op=ALU.mult),
                   reads=["pos_sb"] + [("sel_f", i) for i in range(8)], writes=["pos_sb"])
                op(DVE, lambda: nc.vector.tensor_scalar(out=posm[:], in0=pos_sb[:], scalar1=-1.0, scalar2=None,
                                                        op0=ALU.add), reads=["pos_sb"], writes=["posm"])
                op(DVE, lambda: nc.vector.tensor_copy(out=n_f[:], in_=ps[0:1, bkn, 0:8]), reads=[("ps", bkn)],
                   writes=["n_f"])
                ev_n = op(DVE, lambda: nc.vector.tensor_copy(out=n_i[:], in_=n_f[:]), reads=["n_f"], writes=["n_i"])
                posm_v = posm[:].rearrange("p (i e) -> p i e", e=8)
                units = [(c0, min(4, 28 - c0)) for c0 in range(0, 28, 4)]

                def gather_block(e, jj, j, par, g_):
                    hb = hTe_bufs[par]
                    xa = XH if par == 1 else []
                    with task(128 * jj, g_, par):
                        op(DVE, lambda e=e, jj=jj: nc.vector.tensor_scalar(
                            out=pshift[:], in0=posm_v[:, :, e], scalar1=-128.0 * jj, scalar2=None, op0=ALU.add),
                           reads=["posm"], writes=["pshift"])
                        i0 = min(jj, 7)
                        for i in range(i0, 8):
                            op(DVE, lambda i=i: nc.vector.tensor_scalar(
                                out=Sg[:, i, :], in0=iota_f[:], scalar1=pshift[:, i:i + 1], scalar2=None,
                                op0=ALU.is_equal),
                               reads=["pshift", "iota_f"], writes=[("Sg", i), ("xn", 0)])
                        for half in range(2):
                            bk = bank("s")

                            def mmg(half=half, bk=bk, i0=i0):
                                ins = None
                                for kk in range(4):
                                    k = half * 4 + kk
                                    for i in range(i0, 8):
                                        ins = nc.tensor.matmul(ps[:, bk, kk * 128:(kk + 1) * 128],
                                                               hn[:, i, k * 128:(k + 1) * 128], Sg[:, i, :],
                                                               start=(i == i0), stop=(i == 7))
                                return ins
                            op(PE, mmg, reads=[("hn", i) for i in range(8)] + [("Sg", i) for i in range(i0, 8)] + [("xn", 0)],
                               writes=[("ps", bk)])
                            op(DVE, lambda half=half, bk=bk, j=j, hb=hb: nc.vector.tensor_tensor(
                                out=hb[:, half * 4:(half + 1) * 4, j * 128:(j + 1) * 128],
                                in0=ps[:, bk, :].rearrange("p (k t) -> p k t", k=4),
                                in1=gfm[:, 3, half * 4:(half + 1) * 4].unsqueeze(2).to_broadcast([128, 4, 128]),
                                op=ALU.mult),
                               reads=[("ps", bk), "gfm"], writes=[("hTe", par, j, half)] + xa)

                pending_sc = []
                pending_g = []

                def expert_pass(e, p, coarse):
                    Wg = od_exp_gate[0, e]
                    Wu = od_exp_up[0, e]
                    Wd = od_exp_down[0, e]
                    guard_i = guard and not coarse
                    par = 0 if coarse else (e % 2)
                    hb = hTe_bufs[par]
                    xa = XH if par == 1 else []
                    if True:
                        jj0 = p * BPP
                        if coarse:
                            for j in range(BPP):
                                gather_block(e, jj0 + j, j, 0, guard_i)
                        seq = []
                        slabs = {}
                        for ui, (c0, ncu) in enumerate(units):
                            for j in range(BPP):
                                seq.append((ui, j))
                        nseq = len(seq)
                        stA = {}

                        def load_unit(ui):
                            c0, ncu = units[ui]
                            if coarse or not cfg.get("wcache", True):
                                slabs[ui] = (col_slab(Wg, c0 * 128, ncu * 128), col_slab(Wu, c0 * 128, ncu * 128),
                                             row_slab(Wd, c0 * 128, ncu))
                            elif st == 0:
                                base = (e * 7 + ui) * 3
                                res = [col_slab(Wg, c0 * 128, ncu * 128), col_slab(Wu, c0 * 128, ncu * 128),
                                       row_slab(Wd, c0 * 128, ncu)]
                                flush_wb()
                                for w_, (v_, key_) in enumerate(res):
                                    wb_pending.append((base + w_, key_))
                                slabs[ui] = tuple(res)
                            else:
                                base = (e * 7 + ui) * 3
                                slabs[ui] = (cache_slab(base, True), cache_slab(base + 1, True),
                                             cache_slab(base + 2, False))

                        def stageA(si):
                            ui, j = seq[si]
                            if j == 0:
                                load_unit(ui)
                            (gsv, gsk), (usv, usk), _ = slabs[ui]
                            jx = si % 2
                            with task(128 * (jj0 + j), guard_i, par):
                                bg = bank("g")
                                bu = bank("u")
                                for (sv, sk, bk_) in ((gsv, gsk, bg), (usv, usk, bu)):
                                    def mm(sv=sv, bk_=bk_, j=j):
                                        ins = None
                                        for k in range(8):
                                            ins = nc.tensor.matmul(ps[:, bk_, :], hb[:, k, j * 128:(j + 1) * 128],
                                                                   sv[:, k, 0:512], start=(k == 0), stop=(k == 7))
                                        return ins
                                    op(PE, mm, reads=[sk, ("hTe", par, j, 0), ("hTe", par, j, 1)] + xa, writes=[("ps", bk_)])
                                op(ACT, lambda bg=bg, jx=jx: nc.scalar.activation(out=sgm[:, jx, :], in_=ps[:, bg, :],
                                                                                  func=AF.Silu),
                                   reads=[("ps", bg)], writes=[("sgm", jx)], force=(not coarse))
                                op(DVE, lambda bu=bu, jx=jx: nc.vector.tensor_tensor(out=a_sl[:, jx, :], in0=ps[:, bu, :],
                                                                                     in1=sgm[:, jx, :], op=ALU.mult),
                                   reads=[("ps", bu), ("sgm", jx)], writes=[("a_sl", jx)])

                        def stageB(si):
                            ui, j = seq[si]
                            jx = si % 2
                            with task(128 * (jj0 + j), guard_i, par):
                                def tr(jx=jx):
                                    ins = None
                                    for c in range(4):
                                        ins = nc.tensor.transpose(psb[:, 0, c * 128:(c + 1) * 128],
                                                                  a_sl[:, jx, c * 128:(c + 1) * 128], identb[:])
                                    return ins
                                op(PE, tr, reads=[("a_sl", jx), "identb"], writes=[("psb", 0)])
                                op(DVE, lambda jx=jx: nc.vector.tensor_copy(out=aTe[:, jx, :], in_=psb[:, 0, 0:512]),
                                   reads=[("psb", 0)], writes=[("aTe", jx)])

                        def stageC(si):
                            ui, j = seq[si]
                            jx = si % 2
                            _, _, (dsv, dsk) = slabs[ui]
                            with task(128 * (jj0 + j), guard_i, par):
                                for dh in range(2):
                                    bd = 4 + dh

                                    def mm(dh=dh, bd=bd, jx=jx, dsv=dsv):
                                        ins = None
                                        for c in range(4):
                                            ins = nc.tensor.matmul(ps[:, bd, :], aTe[:, jx, c * 128:(c + 1) * 128],
                                                                   dsv[:, c, dh * 512:(dh + 1) * 512],
                                                                   start=(c == 0), stop=(c == 3))
                                        return ins
                                    op(PE, mm, reads=[dsk, ("aTe", jx)], writes=[("ps", bd)])
                                    osl = o_sl[:, j, dh * 512:(dh + 1) * 512]
                                    if ui == 0:
                                        op(DVE, lambda osl=osl, bd=bd: nc.vector.tensor_copy(out=osl, in_=ps[:, bd, :]),
                                           reads=[("ps", bd)], writes=[("o_sl", j, dh)])
                                    else:
                                        op(DVE, lambda osl=osl, bd=bd: nc.vector.tensor_tensor(
                                            out=osl, in0=ps[:, bd, :], in1=osl, op=ALU.add),
                                           reads=[("ps", bd), ("o_sl", j, dh)], writes=[("o_sl", j, dh)])

                        for step in range(nseq + 2):
                            if step < nseq:
                                stageA(step)
                            if 0 <= step - 1 < nseq:
                                stageB(step - 1)
                            if 0 <= step - 2 < nseq:
                                stageC(step - 2)
                            if (not coarse) and pending_sc and step >= 2:
                                pending_sc.pop(0)()
                            if (not coarse) and pending_g and step >= 18 and step % 2 == 0:
                                pending_g.pop(0)()
                        if not coarse:
                            while pending_sc:
                                pending_sc.pop(0)()
                            while pending_g:
                                pending_g.pop(0)()
                            for i in range(8):
                                d = i % 2
                                op(DVE, lambda i=i, d=d, e=e: nc.vector.tensor_scalar(
                                    out=Dd[:, d, :], in0=identf[:], scalar1=posm_v[:, i, e:e + 1], scalar2=None,
                                    op0=ALU.mult),
                                   reads=["posm", "identf"], writes=[("Dd", d)])
                                op(PE, lambda i=i, d=d: nc.tensor.matmul(
                                    ps[:, 4 + i // 4, (i % 4) * 128:(i % 4 + 1) * 128], onesf[:], Dd[:, d, :],
                                    start=True, stop=True),
                                   reads=[("Dd", d), "onesf"], writes=[("ps", 4 + i // 4)])
                            for j in range(BPP):
                                op(DVE, lambda j=j: nc.vector.tensor_scalar(
                                    out=STj4[:, j, j * 128:], in0=ps[:, 4:6, :].rearrange("p a n -> p (a n)")[:, j * 128:],
                                    scalar1=iotacol[:, j:j + 1], scalar2=None, op0=ALU.is_equal),
                                   reads=[("ps", 4), ("ps", 5), "iotacol"], writes=[("STj4", j)] + HTK)
                                op(DVE, lambda j=j: nc.vector.tensor_copy(out=o_bf4[:, j, :], in_=o_sl[:, j, :]),
                                   reads=[("o_sl", j, 0), ("o_sl", j, 1)], writes=[("o_bf4", j)] + HTK)
                            for i in range(8):
                                for dh in range(2):
                                    def piece(i=i, dh=dh, e=e):
                                        b = i + 2
                                        bk = bank("s")

                                        def mm(bk=bk, i=i, dh=dh):
                                            ins = None
                                            nj = min(BPP, i + 1)
                                            for j in range(nj):
                                                ins = nc.tensor.matmul(ps[:, bk, :], STj4[:, j, i * 128:(i + 1) * 128],
                                                                       o_bf4[:, j, dh * 512:(dh + 1) * 512],
                                                                       start=(j == 0), stop=(j == nj - 1))
                                            return ins
                                        op(PE, mm, reads=[("STj4", j) for j in range(BPP)] + [("o_bf4", j) for j in range(BPP)] + HTK,
                                           writes=[("ps", bk)])
                                        xs = x_sb[:, b, dh * 512:(dh + 1) * 512]
                                        op(DVE, lambda xs=xs, bk=bk, i=i, e=e: nc.vector.scalar_tensor_tensor(
                                            out=xs, in0=ps[:, bk, :], scalar=comb[:, i, e:e + 1], in1=xs,
                                            op0=ALU.mult, op1=ALU.add),
                                           reads=[("ps", bk), ("x", b, dh), ("comb", i)], writes=[("x", b, dh)])
                                    pending_sc.append(piece)
                            return
                        with task(128 * jj0, guard_i):
                            for i in range(8):
                                d = i % 2
                                op(DVE, lambda i=i, d=d, e=e: nc.vector.tensor_scalar(
                                    out=Dd[:, d, :], in0=identf[:], scalar1=posm_v[:, i, e:e + 1], scalar2=None,
                                    op0=ALU.mult),
                                   reads=["posm", "identf"], writes=[("Dd", d)])
                                op(PE, lambda i=i, d=d: nc.tensor.matmul(
                                    ps[:, 4 + i // 4, (i % 4) * 128:(i % 4 + 1) * 128], onesf[:], Dd[:, d, :],
                                    start=True, stop=True),
                                   reads=[("Dd", d), "onesf"], writes=[("ps", 4 + i // 4)])
                        for j in range(BPP):
                            jj = jj0 + j
                            with task(128 * jj, guard_i):
                                op(DVE, lambda jj=jj: nc.vector.tensor_scalar(
                                    out=STj[:], in0=ps[:, 4:6, :].rearrange("p a n -> p (a n)"),
                                    scalar1=iotacol[:, jj:jj + 1], scalar2=None, op0=ALU.is_equal),
                                   reads=[("ps", 4), ("ps", 5), "iotacol"], writes=["STj"])
                                op(DVE, lambda j=j: nc.vector.tensor_copy(out=o_bf[:], in_=o_sl[:, j, :]),
                                   reads=[("o_sl", j, 0), ("o_sl", j, 1)], writes=["o_bf", ("xn", 1)])
                                for i in range(8):
                                    b = i + 2
                                    for dh in range(2):
                                        bk = bank("s")
                                        op(PE, lambda i=i, dh=dh, bk=bk: nc.tensor.matmul(
                                            ps[:, bk, :], STj[:, i * 128:(i + 1) * 128], o_bf[:, dh * 512:(dh + 1) * 512],
                                            start=True, stop=True),
                                           reads=["STj", "o_bf", ("xn", 1)], writes=[("ps", bk)])
                                        xs = x_sb[:, b, dh * 512:(dh + 1) * 512]
                                        op(DVE, lambda xs=xs, bk=bk, i=i, e=e: nc.vector.scalar_tensor_tensor(
                                            out=xs, in0=ps[:, bk, :], scalar=comb[:, i, e:e + 1], in1=xs,
                                            op0=ALU.mult, op1=ALU.add),
                                           reads=[("ps", bk), ("x", b, dh), ("comb", i)], writes=[("x", b, dh)])

                op(DVE, lambda: nc.vector.memset(o_sl[:].rearrange("p j d -> p (j d)"), 0.0),
                   writes=[("o_sl", j_, dh_) for j_ in range(4) for dh_ in range(2)])
                if guard:
                    for E in (PE, DVE):
                        load_reg(E, n_i[0:1, 0:1], ev_n, 0)
                for j in range(BPP):
                    gather_block(0, j, j, 0, guard)
                for e in range(nexp):
                    if e + 1 < nexp:
                        if guard:
                            for E in (PE, DVE):
                                load_reg(E, n_i[0:1, e + 1:e + 2], ev_n, (e + 1) % 2)
                        for j in range(BPP):
                            pending_g.append(lambda e1=e + 1, j=j: gather_block(e1, j, j, e1 % 2, guard))
                    expert_pass(e, 0, False)
                while pending_sc:
                    pending_sc.pop(0)()
                flush_wb()
                for p in range(1, NPASS):
                    for e in range(nexp):
                        if guard:
                            for E in (PE, ACT, DVE, POOL):
                                load_reg(E, n_i[0:1, e:e + 1], ev_n)
                        with task(128 * p * BPP, guard):
                            expert_pass(e, p, True)
            if stop == "moe":
                dump_x(st)
                continue

            blks = B_29
            for b in blks:
                op(ACT, lambda b=b: nc.scalar.activation(out=junk[:], in_=x_sb[:, b, :], func=AF.Square,
                                                         accum_out=ssq[:, b:b + 1]),
                   reads=xkeys([b]), writes=["STj", ("ssq", b)])
            sk = [("ssq", b) for b in blks]
            rk = [("rstd", b) for b in blks]
            op(DVE, lambda: nc.vector.tensor_scalar(out=rstd[:, 2:NB], in0=ssq[:, 2:NB], scalar1=1.0 / D, scalar2=1e-6,
                                                    op0=ALU.mult, op1=ALU.add), reads=sk, writes=rk)
            op(ACT, lambda: nc.scalar.activation(out=rstd[:, 2:NB], in_=rstd[:, 2:NB], func=AF.Sqrt), reads=rk,
               writes=rk)
            op(DVE, lambda: nc.vector.reciprocal(out=rstd[:, 2:NB], in_=rstd[:, 2:NB]), reads=rk, writes=rk)
            for b in blks:
                j = out_i[0] % 2
                out_i[0] += 1
                op(DVE, lambda b=b, j=j: nc.vector.scalar_tensor_tensor(
                    out=ob[:, j, :], in0=x_sb[:, b, :], scalar=rstd[:, b:b + 1], in1=gfin[:], op0=ALU.mult,
                    op1=ALU.mult),
                   reads=xkeys([b]) + [("rstd", b), "gfin"], writes=[("ob", j)])
                r0 = st * T + (b - 2) * 128
                dma(SP, ost[j], y_d[r0:r0 + 128, :], ob[:, j, :], reads=[("ob", j)])

        for d_ in ost:
            if d_.count:
                SP.wait((d_.sem, d_.count, d_))
        for E in (PE, ACT, DVE):
            if E.count:
                SP.wait((E.sem, E.count, E))
        for d_ in slot_sems + wb_sems + xlds + [xld, cst, cst2]:
            if d_.count:
                SP.wait((d_.sem, d_.count, d_))
    return nc


def _host_consts(core):
    b = core // 4
    s0 = (core % 4) * TOK_CORE
    oh = np.zeros((32, 383), np.float32)
    for i, bkt in enumerate(_BT):
        oh[bkt, i + 64] = 1.0
    kmask = np.zeros((128, 4, NB), np.float32)
    corr = np.ones((128, 4, 4, 16), np.float32)
    for st in range(4):
        start = s0 + st * T
        for blk in range(NB):
            if start - HALO + blk * 128 < 0:
                kmask[:, st, blk] = NEG
        for g, w in enumerate(POOL_W):
            for i in range(16):
                t = start + i
                corr[:, st, g, i] = float(w) / float(min(t + 1, w))
    vmask = np.zeros((128, 4), np.float32)
    vmask[64:, 1] = NEG
    vmask[:64, 2] = NEG
    return {
        "c_oh": oh,
        "c_kmask": kmask.reshape(128, 4 * NB),
        "c_corr": corr.reshape(128, 256),
        "c_ident": np.eye(128, dtype=np.float32),
        "c_vmask": vmask,
        "c_ltri": np.triu(np.ones((128, 128), np.float32), 1),
        "c_iota": np.tile(np.arange(128, dtype=np.float32)[None, :], (128, 1)),
        "c_iotacol": (np.arange(128, dtype=np.float32)[:, None] + 128.0 * np.arange(8, dtype=np.float32)[None, :]),
    }


def kernel(**inputs):
    cfg = CFG
    ncores = cfg["ncores"]
    x = np.asarray(inputs["x"], dtype=np.float32)
    shared = {k: np.ascontiguousarray(np.asarray(v, dtype=np.float32)) for k, v in inputs.items() if k != "x"}
    nc = build_nc(cfg)
    in_maps = []
    for c in range(ncores):
        b = c // 4
        s0 = (c % 4) * TOK_CORE
        xs = np.zeros((TOK_CORE + HALO, D), np.float32)
        if s0 == 0:
            xs[HALO:] = x[b, 0:TOK_CORE]
        else:
            xs[:] = x[b, s0 - HALO:s0 + TOK_CORE]
        m = dict(shared)
        m["x"] = xs
        m.update(_host_consts(c))
        in_maps.append(m)
    res = run_bass_kernel_spmd(nc, in_maps, core_ids=list(range(ncores)))
    out = np.zeros((2, SEQ, D), np.float32)
    for c in range(ncores):
        b = c // 4
        s0 = (c % 4) * TOK_CORE
        out[b, s0:s0 + TOK_CORE] = np.asarray(res.results[c]["y"]).reshape(TOK_CORE, D)
    return out
```
